# Optimizing a Trainium2 kernel written in Bass

```python
import math
import jax
import jax.numpy as jnp
from jax import lax
import numpy as np

D_MODEL = 1024
BATCH = 8
SEQ = 2048
DEPTH = 1

GRID_W = 64
CTX_LEN = 256
D_MIX = D_MODEL
HY_WIDTH = D_MIX // 2
RET_WIDTH = D_MIX - HY_WIDTH
RET_HEADS = 4
RET_HEAD_DIM = RET_WIDTH // RET_HEADS
RET_CHUNK = 128
RET_DECAY_OFFSET_F = 5.0
RET_DECAY_OFFSET_B = 5.5
ROPE_AXIS_DIM = RET_HEAD_DIM // 4
ROPE_BASE = 10000.0
HY_EMB_DIM = 33
HY_FILTER_HIDDEN = 64
HY_DECAY_TARGET = 1e-2
HY_FAST_PCT = 0.3
HY_SLOW_PCT = 1.5
HY_DECAY_SHIFT = 0.05
PROJ_COLS = 3 * HY_WIDTH + 4 * RET_WIDTH
N_GROUPS = 4
EXPERTS_PER_GROUP = 4
TOP_K_WITHIN = 2
EXPERT_HIDDEN = D_MODEL // 2
N_MOD = 6
EPS = 1e-6
F32 = jnp.float32

kernel_name = 'hybrid_hyena_retention_hmoe_dit'


def rmsnorm(x, w):
    xf = x.astype(F32)
    y = xf * lax.rsqrt(jnp.mean(xf * xf, axis=-1, keepdims=True) + EPS)
    return (y * w.astype(F32)).astype(x.dtype)


def modulate(h, shift, scale):
    return h * (1.0 + scale) + shift


def short_conv3(u, w, b):
    up = jnp.pad(u, ((0, 0), (1, 1), (0, 0)))
    return up[:, :-2] * w[0] + up[:, 1:-1] * w[1] + up[:, 2:] * w[2] + b


def hyena_filters(n_tok, lp):
    t = jnp.linspace(0.0, 1.0, n_tok, dtype=F32)[:, None]
    n_bands = (HY_EMB_DIM - 1) // 2
    bands = jnp.linspace(1e-4, n_bands - 1, n_bands, dtype=F32)
    ang = (2.0 * math.pi / n_tok) * jnp.arange(n_tok, dtype=F32)[:, None] * bands[None, :]
    z = jnp.concatenate([t, jnp.cos(ang), -jnp.sin(ang)], axis=-1)
    freq = lp['hy_f_freq'].astype(F32)
    h = jnp.sin(freq * (z @ lp['hy_f_w1'].astype(F32) + lp['hy_f_b1'].astype(F32)))
    h = jnp.sin(freq * (h @ lp['hy_f_w2'].astype(F32) + lp['hy_f_b2'].astype(F32)))
    h = jnp.sin(freq * (h @ lp['hy_f_w3'].astype(F32) + lp['hy_f_b3'].astype(F32)))
    h = h @ lp['hy_f_wout'].astype(F32)
    max_decay = math.log(HY_DECAY_TARGET) / HY_FAST_PCT
    min_decay = math.log(HY_DECAY_TARGET) / HY_SLOW_PCT
    deltas = jnp.abs(jnp.linspace(min_decay, max_decay, HY_WIDTH, dtype=F32))
    window = jnp.exp(-t * deltas[None, :]) + HY_DECAY_SHIFT
    h = h.reshape(n_tok, 2, HY_WIDTH) * window[:, None, :]
    return h[:, 0], h[:, 1]


def bidir_long_conv(u, h_fwd, h_bwd, skip):
    n_tok = u.shape[1]
    n_fft = 2 * n_tok
    filt = jnp.concatenate([h_fwd, jnp.zeros_like(h_fwd[:1]), h_bwd[:0:-1]], axis=0)
    uf = u.astype(F32)
    y = jnp.fft.irfft(jnp.fft.rfft(uf, n=n_fft, axis=1) * jnp.fft.rfft(filt, n=n_fft, axis=0)[None],
                      n=n_fft, axis=1)[:, :n_tok]
    return (y + uf * skip.astype(F32)).astype(u.dtype)


def hyena_mixer(p_hy, lp):
    u = short_conv3(p_hy, lp['hy_conv_w'], lp['hy_conv_b'])
    x0, x1, v = jnp.split(u, 3, axis=-1)
    h_fwd, h_bwd = hyena_filters(u.shape[1], lp)
    y = x0 * bidir_long_conv(v * x1, h_fwd, h_bwd, lp['hy_skip'])
    return rmsnorm(y, lp['hy_out_norm'])


def split_heads(t):
    b, n_tok, _ = t.shape
    return t.reshape(b, n_tok, RET_HEADS, RET_HEAD_DIM).transpose(0, 2, 1, 3)


def grid_positions(n_tok):
    ROWS = n_tok // GRID_W
    rows = jnp.repeat(jnp.arange(ROWS, dtype=F32), GRID_W)
    cols = jnp.tile(jnp.arange(GRID_W, dtype=F32), ROWS)
    return rows, cols


def axial_rope(t, rows, cols):
    n_freq = ROPE_AXIS_DIM // 2
    freqs = ROPE_BASE ** (-jnp.arange(n_freq, dtype=F32) / n_freq)

    def rot(seg, pos):
        ang = pos[:, None] * freqs[None, :]
        cs, sn = jnp.cos(ang), jnp.sin(ang)
        a, b = seg[..., :n_freq], seg[..., n_freq:]
        return jnp.concatenate([a * cs - b * sn, a * sn + b * cs], axis=-1)

    return jnp.concatenate([rot(t[..., :ROPE_AXIS_DIM], rows),
                            rot(t[..., ROPE_AXIS_DIM:2 * ROPE_AXIS_DIM], cols),
                            t[..., 2 * ROPE_AXIS_DIM:]], axis=-1)


def log_gammas(offset):
    return jnp.log1p(-jnp.exp2(-(offset + jnp.arange(RET_HEADS, dtype=F32))))


def decay_tables(log_gamma):
    i = jnp.arange(RET_CHUNK, dtype=F32)
    diff = i[:, None] - i[None, :]
    d_intra = jnp.where(diff >= 0, jnp.exp(jnp.maximum(diff, 0.0)[None] * log_gamma[:, None, None]), 0.0)
    xi = jnp.exp((i + 1.0)[None, :] * log_gamma[:, None])
    zeta = jnp.exp((RET_CHUNK - 1.0 - i)[None, :] * log_gamma[:, None])
    g_chunk = jnp.exp(RET_CHUNK * log_gamma)
    return d_intra, xi, zeta, g_chunk


def retention_scan(q, k, v, log_gamma, s0):
    b, nh, n_tok, dh = q.shape
    n_chunks = n_tok // RET_CHUNK
    d_intra, xi, zeta, g_chunk = decay_tables(log_gamma)

    def chunks(t):
        return t.reshape(b, nh, n_chunks, RET_CHUNK, dh).transpose(2, 0, 1, 3, 4)

    def step(s, qkv):
        qc, kc, vc = qkv
        scores = jnp.einsum('bhid,bhjd->bhij', qc, kc) * d_intra[None]
        inner = jnp.einsum('bhij,bhjd->bhid', scores, vc)
        cross = jnp.einsum('bhid,bhde->bhie', qc, s) * xi[None, :, :, None]
        s_new = s * g_chunk[None, :, None, None] + jnp.einsum('bhjd,bhje->bhde', kc * zeta[None, :, :, None], vc)
        return s_new, inner + cross

    s_fin, o = lax.scan(step, s0, (chunks(q), chunks(k), chunks(v)))
    return o.transpose(1, 2, 0, 3, 4).reshape(b, nh, n_tok, dh), s_fin


def decayed_state(k, v, log_gamma):
    n_tok = k.shape[2]
    w = jnp.exp((n_tok - 1.0 - jnp.arange(n_tok, dtype=F32))[None, :] * log_gamma[:, None])
    return jnp.einsum('hl,bhld,bhle->bhde', w, k, v)


def head_group_norm(o, w):
    mu = jnp.mean(o, axis=-1, keepdims=True)
    var = jnp.mean(jnp.square(o - mu), axis=-1, keepdims=True)
    on = (o - mu) * lax.rsqrt(var + EPS)
    b, nh, n_tok, dh = o.shape
    return on.transpose(0, 2, 1, 3).reshape(b, n_tok, nh * dh) * w.astype(F32)


def token_mixer(h, lp, s0_f, s0_b, on_grid):
    proj = h @ lp['w_in']
    y_hy = hyena_mixer(proj[..., :3 * HY_WIDTH], lp)
    q, k, v, g = jnp.split(proj[..., 3 * HY_WIDTH:], 4, axis=-1)
    q, k, v = (split_heads(t).astype(F32) for t in (q, k, v))
    k = k * RET_HEAD_DIM ** -0.5
    if on_grid:
        rows, cols = grid_positions(h.shape[1])
        q = axial_rope(q, rows, cols)
        k = axial_rope(k, rows, cols)
    o_f, s_f = retention_scan(q, k, v, log_gammas(RET_DECAY_OFFSET_F), s0_f)
    o_b, s_b = retention_scan(q[:, :, ::-1], k[:, :, ::-1], v[:, :, ::-1], log_gammas(RET_DECAY_OFFSET_B), s0_b)
    o = o_f + o_b[:, :, ::-1]
    y_ret = (jax.nn.silu(g.astype(F32)) * head_group_norm(o, lp['ret_gn_w'])).astype(h.dtype)
    y = jnp.concatenate([y_hy, y_ret], axis=-1) @ lp['w_out']
    return y, s_f, s_b


def context_states(hc, lp):
    w_in = lp['w_in']
    k0 = 3 * HY_WIDTH + RET_WIDTH
    k = split_heads(hc @ w_in[:, k0:k0 + RET_WIDTH]).astype(F32) * RET_HEAD_DIM ** -0.5
    v = split_heads(hc @ w_in[:, k0 + RET_WIDTH:k0 + 2 * RET_WIDTH]).astype(F32)
    s_f = decayed_state(k, v, log_gammas(RET_DECAY_OFFSET_F))
    s_b = decayed_state(k[:, :, ::-1], v[:, :, ::-1], log_gammas(RET_DECAY_OFFSET_B))
    return s_f, s_b


def hier_moe(h, lp):
    b, n_tok, d = h.shape
    t = h.reshape(-1, d)
    tf = t.astype(F32)
    lg = tf @ lp['router_g_w'].astype(F32) + lp['router_g_b'].astype(F32)
    pg = jax.nn.softmax(lg, axis=-1)
    onehot_g = jax.nn.one_hot(jnp.argmax(lg, axis=-1), N_GROUPS, dtype=F32)
    p_sel = jnp.sum(pg * onehot_g, axis=-1)
    le = (tf @ lp['router_e_w'].astype(F32) + lp['router_e_b'].astype(F32)).reshape(-1, N_GROUPS, EXPERTS_PER_GROUP)
    pe = jax.nn.softmax(jnp.einsum('tg,tge->te', onehot_g, le), axis=-1)
    top_v, top_i = lax.top_k(pe, TOP_K_WITHIN)
    top_v = top_v / jnp.sum(top_v, axis=-1, keepdims=True)
    w_within = jnp.einsum('tk,tke->te', top_v, jax.nn.one_hot(top_i, EXPERTS_PER_GROUP, dtype=F32))
    combine = (onehot_g[:, :, None] * (p_sel[:, None] * w_within)[:, None, :]).astype(t.dtype)
    y = jnp.zeros_like(t)
    for gi in range(N_GROUPS):
        a = jnp.einsum('td,edh->teh', t, lp['exp_w1'][gi])
        u = jnp.einsum('td,edh->teh', t, lp['exp_w3'][gi])
        hid = jax.nn.silu(a) * u * combine[:, gi, :, None]
        y = y + jnp.einsum('teh,ehd->td', hid, lp['exp_w2'][gi])
    return y.reshape(b, n_tok, d)


def setup_inputs(seed: int = 0) -> dict:
    key = jax.random.key(seed)
    ks = jax.random.split(key, 32)
    D = D_MODEL

    def nrm(k, shape, scale):
        return scale * jax.random.normal(k, shape, F32)

    return {
        'x': nrm(ks[0], (BATCH, SEQ, D), 1.0),
        'c': nrm(ks[1], (BATCH, D), 1.0),
        'ctx': nrm(ks[2], (BATCH, CTX_LEN, D), 1.0),
        'c_ctx': nrm(ks[3], (D,), 1.0),
        'ada_w': nrm(ks[4], (DEPTH, D, N_MOD * D), 0.5 * D ** -0.5),
        'ada_b': nrm(ks[5], (DEPTH, N_MOD * D), 0.02),
        'norm1_w': 1.0 + nrm(ks[6], (DEPTH, D), 0.05),
        'w_in': nrm(ks[7], (DEPTH, D, PROJ_COLS), D ** -0.5),
        'hy_conv_w': nrm(ks[8], (DEPTH, 3, 3 * HY_WIDTH), 3 ** -0.5),
        'hy_conv_b': nrm(ks[9], (DEPTH, 3 * HY_WIDTH), 0.02),
        'hy_f_w1': nrm(ks[10], (DEPTH, HY_EMB_DIM, HY_FILTER_HIDDEN), HY_EMB_DIM ** -0.5),
        'hy_f_b1': nrm(ks[11], (DEPTH, HY_FILTER_HIDDEN), 0.1),
        'hy_f_w2': nrm(ks[12], (DEPTH, HY_FILTER_HIDDEN, HY_FILTER_HIDDEN), HY_FILTER_HIDDEN ** -0.5),
        'hy_f_b2': nrm(ks[13], (DEPTH, HY_FILTER_HIDDEN), 0.1),
        'hy_f_w3': nrm(ks[14], (DEPTH, HY_FILTER_HIDDEN, HY_FILTER_HIDDEN), HY_FILTER_HIDDEN ** -0.5),
        'hy_f_b3': nrm(ks[15], (DEPTH, HY_FILTER_HIDDEN), 0.1),
        'hy_f_wout': nrm(ks[16], (DEPTH, HY_FILTER_HIDDEN, 2 * HY_WIDTH), HY_FILTER_HIDDEN ** -0.5),
        'hy_f_freq': 1.0 + nrm(ks[17], (DEPTH, HY_FILTER_HIDDEN), 0.1),
        'hy_skip': nrm(ks[18], (DEPTH, HY_WIDTH), 0.5),
        'hy_out_norm': 1.0 + nrm(ks[19], (DEPTH, HY_WIDTH), 0.05),
        'ret_gn_w': 1.0 + nrm(ks[20], (DEPTH, RET_WIDTH), 0.05),
        'w_out': nrm(ks[21], (DEPTH, D_MIX, D), D_MIX ** -0.5),
        'norm2_w': 1.0 + nrm(ks[22], (DEPTH, D), 0.05),
        'router_g_w': nrm(ks[23], (DEPTH, D, N_GROUPS), D ** -0.5),
        'router_g_b': nrm(ks[24], (DEPTH, N_GROUPS), 0.01),
        'router_e_w': nrm(ks[25], (DEPTH, D, N_GROUPS * EXPERTS_PER_GROUP), D ** -0.5),
        'router_e_b': nrm(ks[26], (DEPTH, N_GROUPS * EXPERTS_PER_GROUP), 0.01),
        'exp_w1': nrm(ks[27], (DEPTH, N_GROUPS, EXPERTS_PER_GROUP, D, EXPERT_HIDDEN), D ** -0.5),
        'exp_w3': nrm(ks[28], (DEPTH, N_GROUPS, EXPERTS_PER_GROUP, D, EXPERT_HIDDEN), D ** -0.5),
        'exp_w2': nrm(ks[29], (DEPTH, N_GROUPS, EXPERTS_PER_GROUP, EXPERT_HIDDEN, D), EXPERT_HIDDEN ** -0.5),
        'final_norm_w': 1.0 + nrm(ks[30], (D,), 0.05),
    }


def reference(x, c, ctx, c_ctx, ada_w, ada_b, norm1_w, w_in, hy_conv_w, hy_conv_b, hy_f_w1, hy_f_b1,
              hy_f_w2, hy_f_b2, hy_f_w3, hy_f_b3, hy_f_wout, hy_f_freq, hy_skip, hy_out_norm, ret_gn_w,
              w_out, norm2_w, router_g_w, router_g_b, router_e_w, router_e_b, exp_w1, exp_w3, exp_w2,
              final_norm_w):
    b = x.shape[0]
    for i in range(DEPTH):
        last = i == DEPTH - 1
        lp = {
            'w_in': w_in[i], 'hy_conv_w': hy_conv_w[i], 'hy_conv_b': hy_conv_b[i],
            'hy_f_w1': hy_f_w1[i], 'hy_f_b1': hy_f_b1[i], 'hy_f_w2': hy_f_w2[i], 'hy_f_b2': hy_f_b2[i],
            'hy_f_w3': hy_f_w3[i], 'hy_f_b3': hy_f_b3[i], 'hy_f_wout': hy_f_wout[i], 'hy_f_freq': hy_f_freq[i],
            'hy_skip': hy_skip[i], 'hy_out_norm': hy_out_norm[i], 'ret_gn_w': ret_gn_w[i], 'w_out': w_out[i],
            'router_g_w': router_g_w[i], 'router_g_b': router_g_b[i], 'router_e_w': router_e_w[i],
            'router_e_b': router_e_b[i], 'exp_w1': exp_w1[i], 'exp_w3': exp_w3[i], 'exp_w2': exp_w2[i],
        }
        mod = jax.nn.silu(c) @ ada_w[i] + ada_b[i]
        sh1, sc1, g1, sh2, sc2, g2 = [m[:, None, :] for m in jnp.split(mod, N_MOD, axis=-1)]
        mod_c = jax.nn.silu(c_ctx) @ ada_w[i] + ada_b[i]
        csh1, csc1, cg1, csh2, csc2, cg2 = jnp.split(mod_c, N_MOD, axis=-1)

        hc = modulate(rmsnorm(ctx, norm1_w[i]), csh1, csc1)
        if last:
            s_f, s_b = context_states(hc, lp)
        else:
            zero_state = jnp.zeros((b, RET_HEADS, RET_HEAD_DIM, RET_HEAD_DIM), F32)
            yc, s_f, s_b = token_mixer(hc, lp, zero_state, zero_state, False)
            ctx = ctx + cg1 * yc
            ctx = ctx + cg2 * hier_moe(modulate(rmsnorm(ctx, norm2_w[i]), csh2, csc2), lp)

        h = modulate(rmsnorm(x, norm1_w[i]), sh1, sc1)
        y, _, _ = token_mixer(h, lp, s_f, s_b, True)
        x = x + g1 * y
        x = x + g2 * hier_moe(modulate(rmsnorm(x, norm2_w[i]), sh2, sc2), lp)
    return rmsnorm(x, final_norm_w)
```

```python
import contextlib
import math
import numpy as np
import ml_dtypes
import concourse.bass as bass
import concourse.mybir as mybir
from concourse.bass_utils import run_bass_kernel_spmd

F32 = mybir.dt.float32
BF16 = mybir.dt.bfloat16
AF = mybir.ActivationFunctionType
ALU = mybir.AluOpType
AX = mybir.AxisListType

D = 1024
L = 2048
NT = 16
CTX = 256
EPS = 1e-6
MAGIC = 12582912.0
TWO_PI = 2.0 * math.pi
NFFT = 4096


class Sched:
    def __init__(self, nc, n_dma_sems=12):
        self.nc = nc
        self.ops = []
        self.last_w = {}
        self.readers = {}
        self.n_dma_sems = n_dma_sems
        self.fence = set()
        self.since_fence = {}
        self.cur_cond = None
        self.else_fn = {}
        self.cond_engs = ("pe",)
        self.n_cond = 0
        self.cond_src = {}

    def add(self, eng, fn, reads=(), writes=(), dma=False):
        oid = len(self.ops)
        deps = set(self.fence)
        for r in reads:
            if r in self.last_w:
                deps.add(self.last_w[r])
            if isinstance(r, tuple) and r[0] == "ps":
                deps.update(o for o in self.readers.get(r, ()) if self.ops[o]["eng"] != eng)
        for r in writes:
            if r in self.last_w:
                deps.add(self.last_w[r])
            deps.update(self.readers.get(r, ()))
        self.ops.append(dict(eng=eng, fn=fn, deps=deps, dma=dma, cond=(self.cur_cond if (eng in self.cond_engs and not dma) else None)))
        for r in reads:
            self.readers.setdefault(r, set()).add(oid)
        for r in writes:
            self.last_w[r] = oid
            self.readers[r] = set()
        if dma:
            self.since_fence[("dma", oid)] = oid
        else:
            self.since_fence[eng] = oid
        return oid

    def cond_begin(self, src_ap):
        self.n_cond += 1
        self.cur_cond = self.n_cond
        self.cond_src[self.cur_cond] = src_ap

    def cond_end(self):
        self.cur_cond = None

    def regload(self, eng, key, ap, n):
        def fn(eo):
            regs = self._regbank(eo, (eng, key), n)
            return eo.reg_load(regs, ap)
        return self.add(eng, fn, (), ())

    def _regbank(self, eo, key, n):
        if key not in self._regs:
            self._regs[key] = [self._stack.enter_context(eo.register(f"cr_{key[0]}_{key[1]}_{j}")) for j in range(n)]
        return self._regs[key]

    def barrier(self):
        self.fence = set(self.since_fence.values())
        self.since_fence = {}
        self.last_w = {}
        self.readers = {}

    def pe(self, fn, reads=(), writes=()):
        return self.add("pe", fn, reads, writes)

    def act(self, fn, reads=(), writes=()):
        return self.add("act", fn, reads, writes)

    def dve(self, fn, reads=(), writes=()):
        return self.add("dve", fn, reads, writes)

    def pool(self, fn, reads=(), writes=()):
        return self.add("pool", fn, reads, writes)

    def dma(self, out, in_, reads=(), writes=(), eng="sp", **kw):
        return self.add(eng, lambda e: e.dma_start(out=out, in_=in_, **kw), reads, writes, dma=True)

    def emit(self, stack):
        nc = self.nc
        self._stack = stack
        self._regs = {}
        ops = self.ops
        engs = ["pe", "act", "dve", "pool", "sp"]
        esem = {e: stack.enter_context(nc.semaphore("sem_" + e)) for e in engs}
        dsem = {e: [stack.enter_context(nc.semaphore(f"dsem_{e}{i}")) for i in range(self.n_dma_sems)]
                for e in ("sp", "pool", "act")}

        def skip(pd, o):
            return (not pd["dma"]) and pd["eng"] == o["eng"] and (not o["dma"]) and o["eng"] == "pe"

        target = [False] * len(ops)
        for o in ops:
            for d in o["deps"]:
                pd = ops[d]
                if pd["dma"] or skip(pd, o):
                    continue
                target[d] = True
        cnt = {e: 0 for e in engs}
        dcnt = {e: 0 for e in dsem}
        dval = {e: [0] * self.n_dma_sems for e in dsem}
        token = {}
        prewait = {}
        for i, o in enumerate(ops):
            e = o["eng"]
            if o["dma"]:
                k = dcnt[e] % self.n_dma_sems
                dcnt[e] += 1
                prev = dval[e][k]
                if prev:
                    prewait[i] = (dsem[e][k], prev)
                dval[e][k] = prev + 16
                token[i] = (dsem[e][k], prev + 16)
            elif target[i]:
                cnt[e] += 1
                token[i] = (esem[e], cnt[e])
        per_eng = {e: [] for e in engs}
        seen = {e: {} for e in engs}
        cond_seen = {e: None for e in engs}
        cond_cur = {e: None for e in engs}
        for i, o in enumerate(ops):
            e = o["eng"]
            need = {}
            if o["cond"] != cond_cur[e]:
                cond_cur[e] = o["cond"]
                cond_seen[e] = dict(seen[e]) if cond_cur[e] is not None else None
            seen_e = cond_seen[e] if cond_cur[e] is not None else seen[e]

            def want(s, v):
                key = id(s)
                if need.get(key, (None, 0))[1] < v:
                    need[key] = (s, v)

            for d in o["deps"]:
                pd = ops[d]
                if skip(pd, o):
                    continue
                want(*token[d])
            if i in prewait:
                want(*prewait[i])
            waits = []
            for key, (s, v) in need.items():
                if seen_e.get(key, 0) >= v:
                    continue
                seen_e[key] = v
                waits.append((s, v))
            per_eng[e].append((o, waits, token.get(i)))
        final_waits = []
        for e in dsem:
            for k in range(self.n_dma_sems):
                if dval[e][k]:
                    final_waits.append((dsem[e][k], dval[e][k]))
        self.stats = {e: len(per_eng[e]) for e in engs}
        self.nwaits = {e: sum(len(w) for _, w, _ in per_eng[e]) for e in engs}

        def emit_one(eo, o, waits, tok):
            for s, v in waits:
                eo.wait_ge(s, v)
            inst = o["fn"](eo)
            if tok is not None:
                inst.then_inc(tok[0], 16 if o["dma"] else 1)

        def run(e, eo):
            lst = per_eng[e]
            idx = 0
            creg = None
            while idx < len(lst):
                o, waits, tok = lst[idx]
                if o["cond"] is None:
                    emit_one(eo, o, waits, tok)
                    idx += 1
                    continue
                c = o["cond"]
                j = idx
                while j < len(lst) and lst[j][0]["cond"] == c:
                    j += 1
                ntok = sum(1 for k in range(idx, j) if lst[k][2] is not None)
                src = self.cond_src[c]
                if isinstance(src, tuple):
                    breg = self._regs[(e, src[0])][src[1]]
                else:
                    if creg is None:
                        creg = stack.enter_context(eo.register("condreg_" + e))
                    eo.reg_load(creg, src)
                    breg = creg
                with eo.If_ne(breg, 0):
                    for k in range(idx, j):
                        emit_one(eo, *lst[k])
                if ntok:
                    with eo.Else():
                        if e in self.else_fn:
                            self.else_fn[e](eo).then_inc(esem[e], ntok)
                        else:
                            eo.drain().then_inc(esem[e], ntok)
                idx = j
            if e == "sp":
                for s, v in final_waits:
                    eo.wait_ge(s, v)
                for ee in engs:
                    if ee != "sp" and cnt[ee]:
                        eo.wait_ge(esem[ee], cnt[ee])

        with nc.Block() as block:
            @block.tensor
            def _(eo):
                run("pe", eo)

            @block.scalar
            def _(eo):
                run("act", eo)

            @block.vector
            def _(eo):
                run("dve", eo)

            @block.gpsimd
            def _(eo):
                run("pool", eo)

            @block.sync
            def _(eo):
                run("sp", eo)


class Carver:
    def __init__(self, big, nwords):
        self.big = big
        self.n = nwords
        self.top = 0
        self.peak = 0

    def alloc(self, nelem, dt=F32):
        words = nelem if dt == F32 else (nelem + 1) // 2
        o = self.top
        self.top += words
        self.peak = max(self.peak, self.top)
        assert self.top <= self.n, f"SBUF carve overflow {self.top} > {self.n}"
        v = self.big[:, o:o + words]
        return v if dt == F32 else v.bitcast(dt)

    def mark(self):
        return self.top

    def reset(self, m):
        self.top = m


def bview(ap, off, pat):
    return bass.AP(ap.tensor, ap.offset + off, [list(ap.ap[0])] + [list(p) for p in pat])


_CONST = None


def _bf(a):
    return np.ascontiguousarray(a.astype(ml_dtypes.bfloat16))


def host_constants():
    global _CONST
    if _CONST is not None:
        return _CONST
    c = {}
    t = np.linspace(0.0, 1.0, L, dtype=np.float32)[:, None]
    bands = np.linspace(1e-4, 15, 16, dtype=np.float32)
    ang = (np.float32(2.0 * math.pi / L) * np.arange(L, dtype=np.float32)[:, None]) * bands[None, :]
    z = np.concatenate([t, np.cos(ang), -np.sin(ang)], axis=-1).astype(np.float32)
    c["zT"] = np.ascontiguousarray(z.T)
    max_decay = math.log(1e-2) / 0.3
    min_decay = math.log(1e-2) / 1.5
    deltas = np.abs(np.linspace(min_decay, max_decay, 512, dtype=np.float32))
    window = (np.exp(-t * deltas[None, :]) + 0.05).astype(np.float32)
    tok = np.zeros((16, 128), dtype=np.int64)
    for par in range(2):
        for jb in range(8):
            tok[par * 8 + jb] = 2 * (128 * jb + np.arange(128)) + par
    c["tok16"] = tok
    wF = window[tok]
    wB = wF.copy()
    wB[0, 0, :] = 0.0
    c["winF"] = np.ascontiguousarray(wF)
    c["winB"] = np.ascontiguousarray(wB)
    pf = np.arange(128)
    Fc = np.zeros((8, 128, 16, 128), dtype=np.float32)
    Fs = np.zeros((8, 128, 16, 128), dtype=np.float32)
    for j in range(8):
        phi = 2.0 * np.pi * (128 * j + pf + 0.5) / NFFT
        a = tok.T[:, :, None].astype(np.float64) * phi[None, None, :]
        Fc[j] = np.cos(a)
        Fs[j] = np.sin(a)
    c["Fc"] = _bf(Fc)
    c["Fs"] = _bf(Fs)
    Gc = np.zeros((4, 128, 8, 512), dtype=np.float32)
    Gs = np.zeros((4, 128, 8, 512), dtype=np.float32)
    for oc in range(4):
        par, half = oc // 2, oc % 2
        tt = 2 * (512 * half + np.arange(512)) + par
        for j in range(8):
            phi = 2.0 * np.pi * (128 * j + pf + 0.5) / NFFT
            a = phi[:, None] * tt[None, :].astype(np.float64)
            Gc[oc, :, j, :] = (2.0 / NFFT) * np.cos(a)
            Gs[oc, :, j, :] = -(2.0 / NFFT) * np.sin(a)
    c["Gc"] = _bf(Gc)
    c["Gs"] = _bf(Gs)
    freqs = (10000.0 ** (-np.arange(16, dtype=np.float32) / 16)).astype(np.float32)
    tn = (128 * np.arange(16)[None, :] + np.arange(128)[:, None])
    rows = (tn // 64).astype(np.float32)
    cols = (tn % 64).astype(np.float32)
    ar = rows[:, :, None] * freqs[None, None, :]
    ac = cols[:, :, None] * freqs[None, None, :]
    c["ropeC"] = np.ascontiguousarray(np.concatenate([np.cos(ar), np.cos(ar), np.cos(ac), np.cos(ac)], -1).astype(np.float32))
    c["ropeS"] = np.ascontiguousarray(np.concatenate([np.sin(ar), np.sin(ar), np.sin(ac), np.sin(ac)], -1).astype(np.float32))
    h = np.arange(4, dtype=np.float64)
    lgf = np.log1p(-np.exp2(-(5.0 + h)))
    lgb = np.log1p(-np.exp2(-(5.5 + h)))
    i = np.arange(128, dtype=np.float64)
    sc = 128.0 ** -0.5
    dif = i[None, :] - i[:, None]
    MT = np.zeros((128, 4, 128))
    for hh in range(4):
        MT[:, hh, :] = (np.where(dif >= 0, np.exp(np.maximum(dif, 0) * lgf[hh]), 0.0)
                        + np.where(dif <= 0, np.exp(np.maximum(-dif, 0) * lgb[hh]), 0.0)) * sc
    c["MT"] = MT.astype(np.float32)
    rep = lambda a: np.ascontiguousarray(np.repeat(a[:, :, None], 128, axis=2).reshape(a.shape[0], 512).astype(np.float32))
    c["zf"] = rep(np.exp((127.0 - i)[:, None] * lgf[None, :]) * sc)
    c["zb"] = rep(np.exp(i[:, None] * lgb[None, :]) * sc)
    c["xi"] = np.ascontiguousarray(np.concatenate([np.exp((i + 1.0)[:, None] * lgf[None, :]),
                                                   np.exp((128.0 - i)[:, None] * lgb[None, :])], 1).astype(np.float32))
    c["gtab"] = np.ascontiguousarray(np.stack([rep(np.tile(np.exp(128.0 * lgf)[None, :], (128, 1))),
                                               rep(np.tile(np.exp(128.0 * lgb)[None, :], (128, 1)))], 1))
    m = np.arange(256, dtype=np.float64)
    cwf = rep(np.exp((255.0 - m)[:, None] * lgf[None, :]) * sc).reshape(2, 128, 512)
    cwb = rep(np.exp(m[:, None] * lgb[None, :]) * sc).reshape(2, 128, 512)
    c["ctxw"] = np.ascontiguousarray(np.stack([cwf, cwb], 1).transpose(2, 0, 1, 3))
    xiT = np.stack([np.exp((i + 1.0)[None, :] * lgf[:, None]), np.exp((128.0 - i)[None, :] * lgb[:, None])], 0)
    c["xiT"] = np.ascontiguousarray(np.tile(xiT[None], (128, 1, 1, 1)).astype(np.float32))
    c["tokid"] = np.ascontiguousarray((128 * np.arange(16)[None, :] + np.arange(128)[:, None]).astype(np.float32))
    c["i128"] = np.ascontiguousarray(np.tile(np.repeat(128.0 * np.arange(16), 4)[None, :], (128, 1)).astype(np.float32))
    _CONST = c
    return c


CONST_SPECS = [
    ("zT", [33, L], F32), ("winF", [16, 128, 512], F32), ("winB", [16, 128, 512], F32),
    ("Fc", [8, 128, 16, 128], BF16), ("Fs", [8, 128, 16, 128], BF16),
    ("Gc", [4, 128, 8, 512], BF16), ("Gs", [4, 128, 8, 512], BF16),
    ("ropeC", [128, 16, 64], F32), ("ropeS", [128, 16, 64], F32),
    ("MT", [128, 4, 128], F32), ("zf", [128, 512], F32), ("zb", [128, 512], F32),
    ("xi", [128, 8], F32), ("gtab", [128, 2, 512], F32), ("ctxw", [128, 2, 2, 512], F32),
    ("tokid", [128, 16], F32), ("i128", [128, 64], F32), ("xiT", [128, 2, 4, 128], F32),
]

INPUT_SPECS = [
    ("x", [L, D]), ("ctx", [CTX, D]), ("cc", [128, 8, 2]), ("ada_w", [D, 6 * D]), ("ada_b", [6 * D]),
    ("adab_fm", [128, 48]), ("nw1", [128, 8]), ("nw2", [128, 8]), ("fnw", [D]),
    ("w_in", [D, 3584]), ("convw", [128, 12, 3]), ("convb", [128, 12]),
    ("f_w1", [33, 64]), ("f_w2", [64, 64]), ("f_w3", [64, 64]), ("f_wout", [64, 1024]),
    ("f_b", [64, 3]), ("f_freq", [64, 1]), ("hy_skip", [1, 512]), ("hynw", [128, 4]), ("gnw", [512]),
    ("w_out", [D, D]), ("rw", [D, 20]), ("rb", [20]),
    ("exp_w1", [16, D, 512]), ("exp_w3", [16, D, 512]), ("exp_w2", [16, 512, D]),
]


def build(debug=None, stop_after=99, dev_cut=None):
    nc = bass.Bass("TRN2", target_bir_lowering=False)
    T = {}
    for name, shape in INPUT_SPECS:
        T[name] = nc.dram_tensor(name, list(shape), F32, kind="ExternalInput").ap()
    for name, shape, dt in CONST_SPECS:
        T[name] = nc.dram_tensor("k_" + name, list(shape), dt, kind="ExternalInput").ap()
    out_d = nc.dram_tensor("out", [L, D], F32, kind="ExternalOutput").ap()
    dbg_specs = {}

    with contextlib.ExitStack() as st:
        S = Sched(nc)
        NW = 53000
        big = st.enter_context(nc.sbuf_tensor("big", [128, NW], F32))
        A = Carver(big, NW)
        PS = [st.enter_context(nc.psum_tensor(f"ps{i}", [128, 512], F32)) for i in range(8)]
        PSB = [p.bitcast(BF16) for p in PS]

        def psr(i):
            return ("ps", i)

        def ACT(out, in_, func, reads, writes, **kw):
            return S.act(lambda e: e.activation(out=out, in_=in_, func=func, **kw), reads, writes)

        def TT(out, in0, in1, op, reads, writes, eng="dve"):
            return S.add(eng, lambda e: e.tensor_tensor(out=out, in0=in0, in1=in1, op=op), reads, writes)

        def TS(out, in0, s1, s2, op0, op1, reads, writes, eng="dve", **kw):
            if op1 is None:
                return S.add(eng, lambda e: e.tensor_scalar(out=out, in0=in0, scalar1=s1, scalar2=None, op0=op0, **kw), reads, writes)
            return S.add(eng, lambda e: e.tensor_scalar(out=out, in0=in0, scalar1=s1, scalar2=s2, op0=op0, op1=op1, **kw), reads, writes)

        def STT(out, in0, scalar, in1, op0, op1, reads, writes):
            return S.dve(lambda e: e.scalar_tensor_tensor(out=out, in0=in0, scalar=scalar, in1=in1, op0=op0, op1=op1), reads, writes)

        def CP(out, in_, reads, writes, eng="dve"):
            return S.add(eng, lambda e: e.tensor_copy(out=out, in_=in_), reads, writes)

        def MMG(lst, reads, writes):
            def fn(e):
                inst = None
                for (o, l, r, s0, s1) in lst:
                    inst = e.matmul(o, lhsT=l, rhs=r, start=s0, stop=s1)
                return inst
            return S.pe(fn, reads, writes)

        def TRG(lst, reads, writes):
            def fn(e):
                inst = None
                for (o, i_, idn) in lst:
                    inst = e.transpose(out=o, in_=i_, identity=idn)
                return inst
            return S.pe(fn, reads, writes)

        def DUMP(name, ap, shape, reads):
            if debug is None or name not in debug:
                return
            dt = ap.dtype
            d = nc.dram_tensor("dbg_" + name, list(shape), dt, kind="ExternalOutput").ap()
            S.dma(d, ap, reads=reads)
            dbg_specs[name] = (list(shape), dt)

        def rsqrt_ops(out, in_, n, scale, reads, writes, tmp):
            TS(tmp, in_, scale, EPS, ALU.mult, ALU.add, reads, [writes[0] + "_t"])
            ACT(tmp, tmp, AF.Sqrt, [writes[0] + "_t"], [writes[0] + "_t"])
            S.dve(lambda e: e.reciprocal(out=out, in_=tmp), [writes[0] + "_t"], writes)

        ident = A.alloc(128, BF16)
        identf = A.alloc(128)
        ones_bf = A.alloc(128, BF16)
        ones_f = A.alloc(128)
        cc_t = A.alloc(16).rearrange("p (k c) -> p k c", c=2)
        sc_f = A.alloc(16).rearrange("p (k c) -> p k c", c=2)
        sc_b = A.alloc(16, BF16).rearrange("p (k c) -> p k c", c=2)
        nw1 = A.alloc(8)
        nw2 = A.alloc(8)
        adab = A.alloc(48)
        modc = A.alloc(32)
        modx = A.alloc(32)
        s1 = A.alloc(8); b1 = A.alloc(8); cs1 = A.alloc(8); cb1 = A.alloc(8); s2 = A.alloc(8); b2 = A.alloc(8)
        g1b = A.alloc(1024)
        g2b = A.alloc(1024)
        fnwb = A.alloc(1024)
        comb = A.alloc(256).rearrange("p (i e) -> p i e", e=16)
        hT = A.alloc(8 * L, BF16).rearrange("p (k t) -> p k t", k=8)
        yT = A.alloc(8 * L, BF16).rearrange("p (k t) -> p k t", k=8)
        PERS = A.mark()

        S.pool(lambda e: e.memset(identf, 0.0), writes=["identf"])
        S.pool(lambda e: e.affine_select(out=identf, in_=identf, pattern=[[-1, 128]], compare_op=ALU.not_equal,
                                         fill=1.0, base=0, channel_multiplier=1), reads=["identf"], writes=["identf"])
        CP(ident, identf, ["identf"], ["ident"])
        S.pool(lambda e: e.memset(ones_f, 1.0), writes=["ones_f"])
        CP(ones_bf, ones_f, ["ones_f"], ["ones_bf"])

        S.dma(cc_t, T["cc"], writes=["cc"])
        S.dma(nw1, T["nw1"], writes=["nw1"])
        S.dma(nw2, T["nw2"], writes=["nw2"])
        S.dma(adab, T["adab_fm"], writes=["adab"])
        S.dma(fnwb, bass.AP(T["fnw"].tensor, 0, [[0, 128], [1, 1024]]), writes=["fnwb"])
        ACT(sc_f, cc_t, AF.Silu, ["cc"], ["sc_f"])
        CP(sc_b, sc_f, ["sc_f"], ["sc_b"])
        adaw = [A.alloc(8 * 1024, BF16).rearrange("p (k n) -> p k n", k=8) for _ in range(2)]
        ada_v = T["ada_w"].rearrange("(k p) n -> p k n", p=128)
        fm_slot = {0: 0, 1: 1, 3: 2, 4: 3}

        def ada_dma(j, buf, bi):
            for k in range(8):
                S.dma(buf[:, k, :], ada_v[:, k, j * 1024:(j + 1) * 1024], writes=[(("adaw", bi), k)], eng="pool")

        def ada_mm(j, buf, bi, lhsb_, ab):
            rk = [(("adaw", bi), k) for k in range(8)]
            if j in fm_slot:
                sl = fm_slot[j]
                bank = sl % 2
                lst = []
                for m in range(8):
                    for k in range(8):
                        lst.append((PS[bank][:, m * 2:(m + 1) * 2], buf[:, k, m * 128:(m + 1) * 128], sc_b[:, k, :], k == 0, k == 7))
                MMG(lst, rk + ["sc_b"], [psr(bank)])
                pv = PS[bank][:, 0:16].rearrange("p (m c) -> p m c", c=2)
                TT(modc[:, sl * 8:(sl + 1) * 8], pv[:, :, 0], adab[:, j * 8:(j + 1) * 8], ALU.add, [psr(bank), "adab"], [("modc", sl)])
                TT(modx[:, sl * 8:(sl + 1) * 8], pv[:, :, 1], adab[:, j * 8:(j + 1) * 8], ALU.add, [psr(bank), "adab"], [("modx", sl)])
            else:
                gb = g1b if j == 2 else g2b
                gname = "g1b" if j == 2 else "g2b"
                S.dma(ab, bass.AP(T["ada_b"].tensor, j * 1024, [[0, 128], [1, 1024]]), writes=[gname + "_ab"])
                for half in range(2):
                    bank = 2 + half
                    lst = [(PS[bank][:, :], lhsb_[:, k, :], buf[:, k, half * 512:(half + 1) * 512], k == 0, k == 7) for k in range(8)]
                    MMG(lst, rk + [("lhsb", k) for k in range(8)], [psr(bank)])
                    TT(gb[:, half * 512:(half + 1) * 512], PS[bank][:, :], ab[:, half * 512:(half + 1) * 512], ALU.add,
                       [psr(bank), gname + "_ab"], [(gname, half)])

        ada_dma(0, adaw[0], 0)
        ada_dma(1, adaw[1], 1)
        ada_mm(0, adaw[0], 0, None, None)
        ada_mm(1, adaw[1], 1, None, None)
        STT(s1, modc[:, 8:16], 1.0, nw1, ALU.add, ALU.mult, [("modc", 1), "nw1"], ["s1"])
        CP(b1, modc[:, 0:8], [("modc", 0)], ["b1"])
        STT(cs1, modx[:, 8:16], 1.0, nw1, ALU.add, ALU.mult, [("modx", 1), "nw1"], ["cs1"])
        CP(cb1, modx[:, 0:8], [("modx", 0)], ["cb1"])
        DUMP("s1", s1, [128, 8], ["s1"])
        DUMP("b1", b1, [128, 8], ["b1"])
        S.barrier()
        A.reset(PERS)
        if stop_after <= 0:
            S.emit(st)
            return nc, dbg_specs, S

        def norm_to_T(src_tile, ntiles, sc_ap, bi_ap, dstT, tag, sc_res, junk, xsb, ss, rstd, rtmp):
            ngrp = (ntiles + 3) // 4
            tiles_of = lambda g: list(range(g * 4, min(ntiles, g * 4 + 4)))

            def st_a(g):
                tiles = tiles_of(g)
                for i in tiles:
                    ap, rd = src_tile(i)
                    ACT(junk, ap, AF.Square, rd, [tag + "junk", (tag + "ss", i)], accum_out=ss[:, i:i + 1])
                g0 = tiles[0]
                rsqrt_ops(rstd[:, g0:g0 + len(tiles)], ss[:, g0:g0 + len(tiles)], len(tiles), 1.0 / D, [(tag + "ss", i) for i in tiles],
                          [tag + "rstd%d" % g], rtmp[:, g0:g0 + len(tiles)])

            def st_b(g):
                base = (g % 2) * 4
                for ii, i in enumerate(tiles_of(g)):
                    ap, rd = src_tile(i)
                    xb = xsb[i % 2]
                    TS(xb, ap, rstd[:, i:i + 1], None, ALU.mult, None, rd + [tag + "rstd%d" % g], [(tag + "xs", i % 2)])
                    lst = []
                    for k in range(8):
                        lst.append((PSB[base + k // 2][:, (k % 2) * 512 + ii * 128:(k % 2) * 512 + (ii + 1) * 128],
                                    xb[:, k * 128:(k + 1) * 128], ident))
                    TRG(lst, [(tag + "xs", i % 2), "ident"], [psr(base + kk) for kk in range(4)])

            def st_c(g):
                base = (g % 2) * 4
                nt_ = len(tiles_of(g))
                for k in range(8):
                    o_ = dstT[:, k, g * 512:g * 512 + nt_ * 128]
                    i_ = PSB[base + k // 2][:, (k % 2) * 512:(k % 2) * 512 + nt_ * 128]
                    if k % 2 == 0:
                        ACT(o_, i_, AF.Identity, [psr(base + k // 2)] + sc_res, [(tag + "T", k, g)], scale=sc_ap[:, k:k + 1], bias=bi_ap[:, k:k + 1])
                    else:
                        TS(o_, i_, sc_ap[:, k:k + 1], bi_ap[:, k:k + 1], ALU.mult, ALU.add, [psr(base + k // 2)] + sc_res, [(tag + "T", k, g)])

            sts = [st_a, st_b, st_c]
            for step in range(ngrp + 2):
                for kk in (2, 1, 0):
                    if 0 <= step - kk < ngrp:
                        sts[kk](step - kk)

        hcT = A.alloc(8 * CTX, BF16).rearrange("p (k t) -> p k t", k=8)
        P1M = A.mark()
        xt = [A.alloc(1024) for _ in range(8)]
        junk = A.alloc(1024)
        xsb = [A.alloc(1024, BF16) for _ in range(2)]
        ss = A.alloc(16); rstd = A.alloc(16); rtmp = A.alloc(16)
        ssc = A.alloc(2); rstdc = A.alloc(2); rtmpc = A.alloc(2)
        x_v = T["x"].rearrange("(i p) d -> p i d", p=128)
        c_v = T["ctx"].rearrange("(i p) d -> p i d", p=128)

        def ctx_tile(i):
            return xt[i % 8], [("xt", i % 8)]
        for i in range(2):
            S.dma(xt[i % 8], c_v[:, i, :], writes=[("xt", i % 8)])
        norm_to_T(ctx_tile, 2, cs1, cb1, hcT, "c", ["cs1", "cb1"], junk, xsb, ssc, rstdc, rtmpc)

        def x_tile(i):
            return xt[i % 8], [("xt", i % 8)]
        loaded = set()

        def x_tile_load(i):
            if i not in loaded:
                loaded.add(i)
                S.dma(xt[i % 8], x_v[:, i, :], writes=[("xt", i % 8)])
            return x_tile(i)
        norm_to_T(x_tile_load, 16, s1, b1, hT, "h", ["s1", "b1"], junk, xsb, ss, rstd, rtmp)
        HT_RES = [("hT", k, g) for k in range(8) for g in range(4)]
        DUMP("hT", hT, [128, 8, L], HT_RES)
        DUMP("hcT", hcT, [128, 8, CTX], [("cT", k, 0) for k in range(8)])
        if stop_after <= 1:
            S.emit(st)
            return nc, dbg_specs, S
        S.barrier()
        A.reset(P1M)

        win_v = T["w_in"].rearrange("(k p) n -> p k n", p=128)
        wch = [A.alloc(8 * 512, BF16).rearrange("p (k n) -> p k n", k=8) for _ in range(2)]

        def load_wchunk(slot, col0):
            for k in range(8):
                S.dma(wch[slot][:, k, :], win_v[:, k, col0:col0 + 512], writes=[("wch", slot, k)], eng="pool")
            return [("wch", slot, k) for k in range(8)]

        ytab = yT.rearrange("p k t -> p (k t)")[:, 0:4 * L].bitcast(F32)
        MTt = ytab[:, 0:512].rearrange("p (h i) -> p h i", h=4)
        ropeC = ytab[:, 512:1536].rearrange("p (n f) -> p n f", n=16)
        ropeS = ytab[:, 1536:2560].rearrange("p (n f) -> p n f", n=16)
        zf = ytab[:, 2560:3072]; zb = ytab[:, 3072:3584]
        xi = A.alloc(8)
        gtab = A.alloc(1024).rearrange("p (d n) -> p d n", d=2)
        gnwb = A.alloc(512)
        Sf = A.alloc(512); Sb = A.alloc(512); Stmp = A.alloc(512)
        P2M = A.mark()
        ctxw = A.alloc(2048).rearrange("p (t d n) -> p t d n", t=2, d=2)
        ckf = [A.alloc(512, BF16) for _ in range(2)]
        ckb = [A.alloc(512, BF16) for _ in range(2)]
        cv = [A.alloc(512, BF16) for _ in range(2)]
        for nm, ap in (("MT", MTt), ("ropeC", ropeC), ("ropeS", ropeS), ("zf", zf), ("zb", zb), ("xi", xi), ("gtab", gtab), ("ctxw", ctxw)):
            S.dma(ap, T[nm], writes=[nm])
        S.dma(gnwb, bass.AP(T["gnw"].tensor, 0, [[0, 128], [1, 512]]), writes=["gnwb"])
        rk_k = load_wchunk(0, 2048)
        rk_v = load_wchunk(1, 2560)

        for ci in range(2):
            lst = [(PS[0][:, :], hcT[:, k, ci * 128:(ci + 1) * 128], wch[0][:, k, :], k == 0, k == 7) for k in range(8)]
            MMG(lst, rk_k + [("cT", k, 0) for k in range(8)], [psr(0)])
            lst = [(PS[1][:, :], hcT[:, k, ci * 128:(ci + 1) * 128], wch[1][:, k, :], k == 0, k == 7) for k in range(8)]
            MMG(lst, rk_v + [("cT", k, 0) for k in range(8)], [psr(1)])
            TT(ckf[ci], PS[0][:, :], ctxw[:, ci, 0, :], ALU.mult, [psr(0), "ctxw"], [("ckf", ci)])
            TT(ckb[ci], PS[0][:, :], ctxw[:, ci, 1, :], ALU.mult, [psr(0), "ctxw"], [("ckb", ci)])
            ACT(cv[ci], PS[1][:, :], AF.Copy, [psr(1)], [("cv", ci)])
        for dr, (kk, Sx, nm) in enumerate(((ckf, Sf, "Sf"), (ckb, Sb, "Sb"))):
            lst = []
            for h in range(4):
                for ci in range(2):
                    lst.append((PS[2 + dr][:, h * 128:(h + 1) * 128], kk[ci][:, h * 128:(h + 1) * 128], cv[ci][:, h * 128:(h + 1) * 128], ci == 0, ci == 1))
            MMG(lst, [("ckf", 0), ("ckf", 1), ("ckb", 0), ("ckb", 1), ("cv", 0), ("cv", 1)], [psr(2 + dr)])
            CP(Sx, PS[2 + dr][:, :], [psr(2 + dr)], [nm])
        DUMP("S0f", Sf, [128, 512], ["Sf"])
        DUMP("S0b", Sb, [128, 512], ["Sb"])
        if stop_after <= 1.5:
            S.emit(st)
            return nc, dbg_specs, S
        S.barrier()
        A.reset(P2M)
        kT = A.alloc(4 * L, BF16).rearrange("p (h t) -> p h t", h=4)
        k_tok = A.alloc(16 * 512, BF16).rearrange("p (n c) -> p n c", n=16)
        v_tok = A.alloc(16 * 512, BF16).rearrange("p (n c) -> p n c", n=16)
        SB = A.alloc(16 * 512, BF16).rearrange("p (n c) -> p n c", n=16)
        Sf_bf = A.alloc(512, BF16)
        qk_tok = [A.alloc(512, BF16) for _ in range(2)]
        rt1 = A.alloc(256); rt2 = A.alloc(256)
        kz = [A.alloc(512, BF16) for _ in range(2)]
        qTt = [A.alloc(512, BF16).rearrange("p (h t) -> p h t", h=4) for _ in range(2)]
        masked = [A.alloc(512, BF16).rearrange("p (h i) -> p h i", h=4) for _ in range(2)]
        sg_t = [A.alloc(512) for _ in range(4)]
        qfT = [A.alloc(512, BF16).rearrange("p (h t) -> p h t", h=4) for _ in range(2)]
        qbT = [A.alloc(512, BF16).rearrange("p (h t) -> p h t", h=4) for _ in range(2)]
        xiT = A.alloc(1024).rearrange("p (d h t) -> p d h t", d=2, h=4)
        on_t = A.alloc(512)
        yr_t = [A.alloc(512, BF16) for _ in range(2)]
        bst = A.alloc(24); mv = A.alloc(8); grs = A.alloc(4); grt = A.alloc(4)
        rk_k = [("wch", 0, k) for k in range(8)]
        rk_v = [("wch", 1, k) for k in range(8)]

        def rope(dst_bf, src_ps, n, ps_res, tagw):
            ACT(dst_bf, src_ps, AF.Copy, [ps_res], [tagw])
            srcv = src_ps.rearrange("p (h c) -> p h c", h=4)[:, :, 0:64]
            cb = bview(ropeC, n * 64, [[0, 4], [1, 64]])
            sb_ = bview(ropeS, n * 64, [[0, 4], [1, 64]])
            t1 = rt1.rearrange("p (h c) -> p h c", h=4)
            t2 = rt2.rearrange("p (h c) -> p h c", h=4)
            TT(t1, srcv, cb, ALU.mult, [ps_res, "ropeC"], ["rt1"])
            TT(t2, srcv, sb_, ALU.mult, [ps_res, "ropeS"], ["rt2"])
            d4 = dst_bf.rearrange("p (h x a f) -> p h x a f", h=4, x=4, a=2)
            t14 = rt1.rearrange("p (h x a f) -> p h x a f", h=4, x=2, a=2)
            t24 = rt2.rearrange("p (h x a f) -> p h x a f", h=4, x=2, a=2)
            TT(d4[:, :, 0:2, 0, :], t14[:, :, :, 0, :], t24[:, :, :, 1, :], ALU.subtract, ["rt1", "rt2"], [tagw])
            TT(d4[:, :, 0:2, 1, :], t14[:, :, :, 1, :], t24[:, :, :, 0, :], ALU.add, ["rt1", "rt2"], [tagw])

        CP(SB[:, 15, :], Sb, ["Sb"], [("SB", 15)])
        hres = lambda n: [("hT", k, n // 4) for k in range(8)]
        def sa1(n):
            pk, pv = 0 + (n % 2) * 4, 1 + (n % 2) * 4
            lst = [(PS[pk][:, :], hT[:, k, n * 128:(n + 1) * 128], wch[0][:, k, :], k == 0, k == 7) for k in range(8)]
            MMG(lst, rk_k + hres(n), [psr(pk)])
            lst = [(PS[pv][:, :], hT[:, k, n * 128:(n + 1) * 128], wch[1][:, k, :], k == 0, k == 7) for k in range(8)]
            MMG(lst, rk_v + hres(n), [psr(pv)])
            ACT(v_tok[:, n, :], PS[pv][:, :], AF.Copy, [psr(pv)], [("v_tok", n)])
            rope(k_tok[:, n, :], PS[pk][:, :], n, psr(pk), ("k_tok", n))

        def sa2(n):
            pt = 2 + (n % 2) * 4
            lst = [(PSB[pt][:, h * 128:(h + 1) * 128], k_tok[:, n, h * 128:(h + 1) * 128], ident) for h in range(4)]
            TRG(lst, [("k_tok", n), "ident"], [psr(pt)])
            CP(kT[:, :, n * 128:(n + 1) * 128], PSB[pt][:, 0:512].rearrange("p (h t) -> p h t", h=4), [psr(pt)], [("kT", n)])
            if n > 0:
                TT(kz[n % 2], k_tok[:, n, :], zb, ALU.mult, [("k_tok", n), "zb"], [("kz", n % 2)], eng="pool")

        def sa3(n):
            pkv = 3 + (n % 2) * 4
            if n > 0:
                kzb = kz[n % 2]
                lst = [(PS[pkv][:, h * 128:(h + 1) * 128], kzb[:, h * 128:(h + 1) * 128], v_tok[:, n, h * 128:(h + 1) * 128], True, True) for h in range(4)]
                MMG(lst, [("kz", n % 2), ("v_tok", n)], [psr(pkv)])
                TT(Stmp, Sb, gtab[:, 1, :], ALU.mult, ["Sb", "gtab"], ["Stmp"], eng="pool")
                TT(Sb, PS[pkv][:, :], Stmp, ALU.add, [psr(pkv), "Stmp"], ["Sb"])
                ACT(SB[:, n - 1, :], Sb, AF.Copy, ["Sb"], [("SB", n - 1)])

        stages_a = [sa1, sa2, sa3]
        for step in range(16 + 2):
            for kk in (2, 1, 0):
                if 0 <= step - kk < 16:
                    stages_a[kk](15 - (step - kk))
        DUMP("kT", kT, [128, 4, L], [("kT", n) for n in range(16)])
        DUMP("SB", SB, [128, 16, 512], [("SB", n) for n in range(16)])
        if stop_after <= 1.7:
            S.emit(st)
            return nc, dbg_specs, S

        rk_q = load_wchunk(0, 1536)
        rk_g = load_wchunk(1, 3072)
        S.dma(xiT, T["xiT"], writes=["xiT"])
        ACT(Sf_bf, Sf, AF.Copy, ["Sf"], ["Sf_bf"])

        def sb1(n):
            b = n % 2
            lst = [(PS[0][:, :], hT[:, k, n * 128:(n + 1) * 128], wch[0][:, k, :], k == 0, k == 7) for k in range(8)]
            MMG(lst, rk_q + hres(n), [psr(0)])
            rope(qk_tok[b], PS[0][:, :], n, psr(0), ("q_tok", b))
            lst = [(PS[2][:, :], hT[:, k, n * 128:(n + 1) * 128], wch[1][:, k, :], k == 0, k == 7) for k in range(8)]
            MMG(lst, rk_g + hres(n), [psr(2)])
            ACT(sg_t[n % 4], PS[2][:, :], AF.Silu, [psr(2)], [("sg", n % 4)])

        def sb2(n):
            b = n % 2
            lst = [(PSB[1][:, h * 128:(h + 1) * 128], qk_tok[b][:, h * 128:(h + 1) * 128], ident) for h in range(4)]
            TRG(lst, [("q_tok", b), "ident"], [psr(1)])
            CP(qTt[b], PSB[1][:, 0:512].rearrange("p (h t) -> p h t", h=4), [psr(1)], [("qT", b)])
            if n < 15:
                TT(kz[b], k_tok[:, n, :], zf, ALU.mult, [("k_tok", n), "zf"], [("kz", b)], eng="pool")
            TT(qfT[b], qTt[b], xiT[:, 0, :, :], ALU.mult, [("qT", b), "xiT"], [("qfT", b)], eng="pool")
            TT(qbT[b], qTt[b], xiT[:, 1, :, :], ALU.mult, [("qT", b), "xiT"], [("qbT", b)], eng="pool")

        def sb2b(n):
            b = n % 2
            lst = [(PS[3][:, h * 128:(h + 1) * 128], kT[:, h, n * 128:(n + 1) * 128], qTt[b][:, h, :], True, True) for h in range(4)]
            MMG(lst, [("kT", n), ("qT", b)], [psr(3)])
            TT(masked[b], PS[3][:, :].rearrange("p (h i) -> p h i", h=4), MTt, ALU.mult, [psr(3), "MT"], [("masked", b)])

        def sb3(n):
            b = n % 2
            po = 4 + b
            if n < 15:
                lst = [(PS[7][:, h * 128:(h + 1) * 128], kz[b][:, h * 128:(h + 1) * 128], v_tok[:, n, h * 128:(h + 1) * 128], True, True) for h in range(4)]
                MMG(lst, [("kz", b), ("v_tok", n)], [psr(7)])
            lst = []
            for h in range(4):
                hs = slice(h * 128, (h + 1) * 128)
                lst.append((PS[po][:, hs], masked[b][:, h, :], v_tok[:, n, hs], True, False))
                lst.append((PS[po][:, hs], qfT[b][:, h, :], Sf_bf[:, hs], False, False))
                lst.append((PS[po][:, hs], qbT[b][:, h, :], SB[:, n, hs], False, True))
            MMG(lst, [("masked", b), ("v_tok", n), ("qfT", b), ("qbT", b), "Sf_bf", ("SB", n)], [psr(po)])
            if n < 15:
                TT(Stmp, Sf, gtab[:, 0, :], ALU.mult, ["Sf", "gtab"], ["Stmp"], eng="pool")
                TT(Sf, PS[7][:, :], Stmp, ALU.add, [psr(7), "Stmp"], ["Sf"])
                ACT(Sf_bf, Sf, AF.Copy, ["Sf"], ["Sf_bf"])

        def sb4(n):
            b = n % 2
            po = 4 + b
            for h in range(4):
                S.dve(lambda e, h=h, po=po: e.bn_stats(out=bst[:, h * 6:(h + 1) * 6], in_=PS[po][:, h * 128:(h + 1) * 128]), [psr(po)], [("bst", h)])
                S.dve(lambda e, h=h: e.bn_aggr(out=mv[:, h * 2:(h + 1) * 2], in_=bst[:, h * 6:(h + 1) * 6]), [("bst", h)], [("mv", h)])
            mvv = mv.rearrange("p (h c) -> p h c", c=2)
            rsqrt_ops(grs, mvv[:, :, 1], 4, 1.0, [("mv", h) for h in range(4)], ["grs"], grt)
            for h in range(4):
                TS(on_t[:, h * 128:(h + 1) * 128], PS[po][:, h * 128:(h + 1) * 128], mv[:, 2 * h:2 * h + 1], grs[:, h:h + 1],
                   ALU.subtract, ALU.mult, [psr(po), ("mv", h), "grs"], ["on"])
            TT(on_t, on_t, gnwb, ALU.mult, ["on", "gnwb"], ["on"], eng="pool")
            TT(yr_t[b], on_t, sg_t[n % 4], ALU.mult, ["on", ("sg", n % 4)], [("yr", b)])

        def sb5(n):
            b = n % 2
            lst = [(PSB[6][:, h * 128:(h + 1) * 128], yr_t[b][:, h * 128:(h + 1) * 128], ident) for h in range(4)]
            TRG(lst, [("yr", b), "ident"], [psr(6)])
            ACT(yT[:, 4:8, n * 128:(n + 1) * 128], PSB[6][:, 0:512].rearrange("p (h t) -> p h t", h=4), AF.Copy, [psr(6)], [("yT", "r", n)])

        stages_b = [sb1, sb2, sb2b, sb3, sb4, sb5]
        for step in range(16 + len(stages_b) - 1):
            for k in range(len(stages_b) - 1, -1, -1):
                if 0 <= step - k < 16:
                    stages_b[k](step - k)
        DUMP("yTr", yT[:, 4:8, :], [128, 4, L], [("yT", "r", n) for n in range(16)])
        S.barrier()
        A.reset(PERS)
        if stop_after <= 2:
            S.emit(st)
            return nc, dbg_specs, S

        pq_p = A.alloc(16 * 512, BF16).rearrange("p (n c) -> p n c", n=16)
        pq_q = A.alloc(16 * 512, BF16).rearrange("p (n c) -> p n c", n=16)
        P3A = A.mark()
        zT = A.alloc(L)
        fa = [A.alloc(L) for _ in range(2)]
        ftmp = A.alloc(L)
        fw1 = A.alloc(64); fw2 = A.alloc(64); fw3 = A.alloc(64)
        fwo = A.alloc(1024)
        fb = A.alloc(3); ffr = A.alloc(1); ffb = A.alloc(3)
        skip_t = A.alloc(512)
        winf = [A.alloc(512) for _ in range(2)]
        winb = [A.alloc(512) for _ in range(2)]
        hf_t = A.alloc(512); hb_t = A.alloc(512)
        S.dma(zT[0:33, :], T["zT"], writes=["zT"])
        S.dma(fw1[0:33, :], T["f_w1"], writes=["fw1"])
        S.dma(fw2[0:64, :], T["f_w2"], writes=["fw2"])
        S.dma(fw3[0:64, :], T["f_w3"], writes=["fw3"])
        S.dma(fwo[0:64, :], T["f_wout"], writes=["fwo"])
        S.dma(fb[0:64, :], T["f_b"], writes=["fb"])
        S.dma(ffr[0:64, :], T["f_freq"], writes=["ffr"])
        S.dma(skip_t[0:1, :], T["hy_skip"], writes=["skip"])
        TS(ffb[0:64, :], fb[0:64, :], ffr[0:64, 0:1], None, ALU.mult, None, ["fb", "ffr"], ["ffb"])
        adaw2 = [A.alloc(8 * 1024, BF16).rearrange("p (k n) -> p k n", k=8) for _ in range(2)]
        adabrow2 = [A.alloc(1024) for _ in range(2)]
        lhsb2 = A.alloc(8 * 128, BF16).rearrange("p (k m) -> p k m", k=8)
        for k in range(8):
            ACT(lhsb2[:, k, :], ones_f, AF.Identity, [], [("lhsb", k)], scale=sc_f[:, k, 0:1])
        ada_dma(2, adaw2[0], 0)
        ada_dma(3, adaw2[1], 1)
        layers = [(fw1, 33, zT, "zT", "fw1"), (fw2, 64, fa[0], ("fa", 0), "fw2"), (fw3, 64, fa[1], ("fa", 1), "fw3")]
        for li, (w, kdim, src, sres, wres) in enumerate(layers):
            dst = fa[li % 2]
            dres = ("fa", li % 2)
            for tcn in range(4):
                bank = tcn % 2
                MMG([(PS[bank][0:64, :], w[0:kdim, :], src[0:kdim, tcn * 512:(tcn + 1) * 512], True, True)], [sres, wres], [psr(bank)])
                ACT(dst[0:64, tcn * 512:(tcn + 1) * 512], PS[bank][0:64, :], AF.Identity, [psr(bank), "ffr", "ffb"], [(dres, tcn)],
                    scale=ffr[0:64, 0:1], bias=ffb[0:64, li:li + 1])
            allr = [(dres, tcn) for tcn in range(4)]
            TS(ftmp[0:64, :], dst[0:64, :], 1.0 / TWO_PI, MAGIC, ALU.mult, ALU.add, allr, ["ftmp"])
            TS(ftmp[0:64, :], ftmp[0:64, :], MAGIC, -TWO_PI, ALU.subtract, ALU.mult, ["ftmp"], ["ftmp"])
            TT(dst[0:64, :], dst[0:64, :], ftmp[0:64, :], ALU.add, allr + ["ftmp"], [dres])
            ACT(dst[0:64, :], dst[0:64, :], AF.Sin, [dres], [dres])
        h3 = fa[0]
        ada_mm(2, adaw2[0], 0, lhsb2, adabrow2[0])
        ada_mm(3, adaw2[1], 1, lhsb2, adabrow2[0])
        ada_dma(4, adaw2[0], 0)
        ada_dma(5, adaw2[1], 1)
        DUMP("h3", h3[0:64, :], [64, L], [("fa", 0)])
        for t16 in range(16):
            par, jb = t16 // 8, t16 % 8
            b = t16 % 2
            lhs = bview(h3[0:64, :], par + 256 * jb, [[2, 128]])
            S.dma(winf[b], T["winF"][t16], writes=[("winf", b)])
            S.dma(winb[b], T["winB"][t16], writes=[("winb", b)])
            for half in range(2):
                MMG([(PS[2 + half + 2 * b][:, :], lhs, fwo[0:64, half * 512:(half + 1) * 512], True, True)], [("fa", 0), "fwo"], [psr(2 + half + 2 * b)])
            TT(hf_t, PS[2 + 2 * b][:, :], winf[b], ALU.mult, [psr(2 + 2 * b), ("winf", b)], ["hf"])
            TT(hb_t, PS[3 + 2 * b][:, :], winb[b], ALU.mult, [psr(3 + 2 * b), ("winb", b)], ["hb"])
            if t16 == 0:
                TT(hf_t[0:1, :], hf_t[0:1, :], skip_t[0:1, :], ALU.add, ["hf", "skip"], ["hf"])
            TT(pq_p[:, t16, :], hf_t, hb_t, ALU.add, ["hf", "hb"], [("pq_p", t16)], eng="pool")
            TT(pq_q[:, t16, :], hb_t, hf_t, ALU.subtract, ["hf", "hb"], [("pq_q", t16)], eng="pool")
        ada_mm(4, adaw2[0], 0, lhsb2, adabrow2[1])
        ada_mm(5, adaw2[1], 1, lhsb2, adabrow2[1])
        STT(s2, modc[:, 24:32], 1.0, nw2, ALU.add, ALU.mult, [("modc", 3)], ["s2"])
        CP(b2, modc[:, 16:24], [("modc", 2)], ["b2"])
        DUMP("g1b", g1b, [128, 1024], [("g1b", 0), ("g1b", 1)])
        DUMP("pq_p", pq_p, [128, 16, 512], [("pq_p", t) for t in range(16)])
        DUMP("pq_q", pq_q, [128, 16, 512], [("pq_q", t) for t in range(16)])
        S.barrier()
        A.reset(P3A)
        if stop_after <= 3:
            S.emit(st)
            return nc, dbg_specs, S

        vx_tok = A.alloc(16 * 512, BF16).rearrange("p (n c) -> p n c", n=16)
        P3B = A.mark()
        wch = [A.alloc(8 * 512, BF16).rearrange("p (k n) -> p k n", k=8) for _ in range(2)]
        convw = A.alloc(36).rearrange("p (c t) -> p c t", t=3)
        convb = A.alloc(12)
        S.dma(convw, T["convw"], writes=["convw"])
        S.dma(convb, T["convb"], writes=["convb"])
        pT = [A.alloc(L + 2) for _ in range(2)]
        u_x1 = A.alloc(L)
        u_v = A.alloc(L)
        vxT = A.alloc(4 * L, BF16).rearrange("p (c t) -> p c t", c=4)
        for i in range(2):
            S.dve(lambda e, p=pT[i]: e.memset(p[:, 0:1], 0.0), writes=[("pT", i)])
            S.dve(lambda e, p=pT[i]: e.memset(p[:, L + 1:L + 2], 0.0), writes=[("pT", i)])

        def conv_chunk(slot, cl, cg, dst, dres, pidx, tmp=None, tres=None):
            rk = [("wch", slot, k) for k in range(8)]
            p = pT[pidx]
            for tcn in range(4):
                bank = (tcn % 2) + 2 * pidx
                lst = [(PS[bank][:, :], wch[slot][:, k, cl * 128:(cl + 1) * 128], hT[:, k, tcn * 512:(tcn + 1) * 512], k == 0, k == 7) for k in range(8)]
                MMG(lst, rk + [("hT", k, tcn) for k in range(8)], [psr(bank)])
                ACT(p[:, 1 + tcn * 512:1 + (tcn + 1) * 512], PS[bank][:, :], AF.Copy, [psr(bank)], [("pT", pidx)])
            if tmp is None:
                tmp, tres = dst, dres
            ACT(tmp, p[:, 1:L + 1], AF.Identity, [("pT", pidx), "convw", "convb"], [tres], scale=convw[:, cg, 1:2], bias=convb[:, cg:cg + 1])
            STT(tmp, p[:, 0:L], convw[:, cg, 0:1], tmp, ALU.mult, ALU.add, [("pT", pidx), "convw", tres], [tres])
            STT(dst, p[:, 2:L + 2], convw[:, cg, 2:3], tmp, ALU.mult, ALU.add, [("pT", pidx), "convw", tres], [dres])

        load_wchunk(0, 512)
        load_wchunk(1, 1024)
        for cl in range(4):
            conv_chunk(0, cl, 4 + cl, u_x1, "u_x1", 0)
            conv_chunk(1, cl, 8 + cl, u_v, "u_v", 1)
            TT(vxT[:, cl, :], u_x1, u_v, ALU.mult, ["u_x1", "u_v"], [("vxT", cl)], eng="pool")
        DUMP("vxT", vxT, [128, 4, L], [("vxT", c) for c in range(4)])
        for t16 in range(16):
            par, jb = t16 // 8, t16 % 8
            bank = 4 + (t16 % 4)
            lst = [(PSB[bank][:, c * 128:(c + 1) * 128], bview(vxT, c * L + par + 256 * jb, [[2, 128]]), ident) for c in range(4)]
            TRG(lst, [("vxT", c) for c in range(4)] + ["ident"], [psr(bank)])
            if t16 % 2 == 0:
                ACT(vx_tok[:, t16, :], PSB[bank][:, 0:512], AF.Copy, [psr(bank)], [("vx_tok", t16)])
            else:
                CP(vx_tok[:, t16, :], PSB[bank][:, 0:512], [psr(bank)], [("vx_tok", t16)])
        S.barrier()
        A.reset(P3B)
        if stop_after <= 4:
            S.emit(st)
            return nc, dbg_specs, S

        PQ = A.alloc(8 * 4 * 512, BF16).rearrange("p (j s c) -> p j s c", j=8, s=4)
        PQ_END = A.mark()
        Fb = [(A.alloc(16 * 128, BF16).rearrange("p (t f) -> p t f", t=16), A.alloc(16 * 128, BF16).rearrange("p (t f) -> p t f", t=16)) for _ in range(2)]
        XsB = [[A.alloc(512, BF16) for _ in range(4)] for _ in range(2)]
        HsB = [[A.alloc(512, BF16) for _ in range(4)] for _ in range(2)]
        tmpB = [[A.alloc(512, BF16) for _ in range(4)] for _ in range(2)]
        _oc = [A.alloc(512, BF16) for _ in range(4)]
        oc_t = [_oc, _oc]
        for j in range(8):
            fb_ = j % 2
            Xs, Hs, tmp4, occ = XsB[fb_], HsB[fb_], tmpB[fb_], oc_t[fb_]
            X = lambda k: ("X", fb_, k)
            Hn = lambda k: ("H", fb_, k)
            Tn = lambda k: ("t", fb_, k)
            On = lambda k: ("oc", k)
            Fcj, Fsj = Fb[fb_]
            S.dma(Fcj, T["Fc"][j], writes=[("Fc", fb_)])
            S.dma(Fsj, T["Fs"][j], writes=[("Fs", fb_)])
            for bi, (Ft, fres, t0) in enumerate(((Fcj, ("Fc", fb_), 0), (Fcj, ("Fc", fb_), 8), (Fsj, ("Fs", fb_), 0), (Fsj, ("Fs", fb_), 8))):
                lst = [(PS[bi][:, :], Ft[:, t0 + i, :], vx_tok[:, t0 + i, :], i == 0, i == 7) for i in range(8)]
                MMG(lst, [fres] + [("vx_tok", t0 + i) for i in range(8)], [psr(bi)])
            for bi, (Ft, fres, t0, src, sn) in enumerate(((Fcj, ("Fc", fb_), 0, pq_p, "pq_p"), (Fcj, ("Fc", fb_), 8, pq_p, "pq_p"),
                                                            (Fsj, ("Fs", fb_), 0, pq_q, "pq_q"), (Fsj, ("Fs", fb_), 8, pq_q, "pq_q"))):
                lst = [(PS[4 + bi][:, :], Ft[:, t0 + i, :], src[:, t0 + i, :], i == 0, i == 7) for i in range(8)]
                MMG(lst, [fres] + [(sn, t0 + i) for i in range(8)], [psr(4 + bi)])
            ACT(occ[0], PS[1][:, :], AF.Copy, [psr(1)], [On(0)])
            ACT(occ[1], PS[3][:, :], AF.Copy, [psr(3)], [On(1)])
            TT(Xs[0], PS[0][:, :], occ[0], ALU.add, [psr(0), On(0)], [X(0)])
            TT(Xs[1], PS[0][:, :], occ[0], ALU.subtract, [psr(0), On(0)], [X(1)])
            TT(Xs[2], PS[2][:, :], occ[1], ALU.add, [psr(2), On(1)], [X(2)])
            STT(Xs[3], PS[2][:, :], -1.0, occ[1], ALU.mult, ALU.add, [psr(2), On(1)], [X(3)])
            ACT(occ[2], PS[5][:, :], AF.Copy, [psr(5)], [On(2)])
            ACT(occ[3], PS[7][:, :], AF.Copy, [psr(7)], [On(3)])
            TT(Hs[0], PS[4][:, :], occ[2], ALU.add, [psr(4), On(2)], [Hn(0)])
            TT(Hs[1], PS[4][:, :], occ[2], ALU.subtract, [psr(4), On(2)], [Hn(1)])
            TT(Hs[2], PS[6][:, :], occ[3], ALU.add, [psr(6), On(3)], [Hn(2)])
            STT(Hs[3], PS[6][:, :], -1.0, occ[3], ALU.mult, ALU.add, [psr(6), On(3)], [Hn(3)])
            for fm in range(2):
                Cx, Sx, Hr, Hi = Xs[fm], Xs[2 + fm], Hs[fm], Hs[2 + fm]
                rC, rS, rHr, rHi = X(fm), X(2 + fm), Hn(fm), Hn(2 + fm)
                e1 = "pool" if fm == 0 else "dve"
                TT(tmp4[0], Cx, Hr, ALU.mult, [rC, rHr], [Tn(0)], eng=e1)
                TT(tmp4[1], Sx, Hi, ALU.mult, [rS, rHi], [Tn(1)], eng=e1)
                TT(tmp4[2], Cx, Hi, ALU.mult, [rC, rHi], [Tn(2)], eng="pool")
                TT(tmp4[3], Sx, Hr, ALU.mult, [rS, rHr], [Tn(3)], eng="pool")
                TT(Cx, tmp4[0], tmp4[1], ALU.add, [Tn(0), Tn(1)], [rC], eng=e1)
                TT(Sx, tmp4[2], tmp4[3], ALU.subtract, [Tn(2), Tn(3)], [rS], eng="pool")
            TT(PQ[:, j, 0, :], Xs[0], Xs[1], ALU.add, [X(0), X(1)], [("PQ", j)])
            TT(PQ[:, j, 1, :], Xs[2], Xs[3], ALU.subtract, [X(2), X(3)], [("PQ", j)])
            TT(PQ[:, j, 2, :], Xs[0], Xs[1], ALU.subtract, [X(0), X(1)], [("PQ", j)], eng="pool")
            TT(PQ[:, j, 3, :], Xs[2], Xs[3], ALU.add, [X(2), X(3)], [("PQ", j)], eng="pool")
        DUMP("PQ", PQ, [128, 8, 4, 512], [("PQ", j) for j in range(8)])
        S.barrier()
        if stop_after <= 5:
            S.emit(st)
            return nc, dbg_specs, S

        A.reset(PERS)
        x0T = A.alloc(4 * L, BF16).rearrange("p (c t) -> p c t", c=4)
        wch = [A.alloc(8 * 512, BF16).rearrange("p (k n) -> p k n", k=8)]
        convw = A.alloc(36).rearrange("p (c t) -> p c t", t=3)
        convb = A.alloc(12)
        pT = [A.alloc(L + 2) for _ in range(1)]
        u0 = A.alloc(L)
        Gb = []
        assert A.top <= PERS + 3 * 16 * 256, "P3d scratch must stay below PQ"
        S.dma(convw, T["convw"], writes=["convw"])
        S.dma(convb, T["convb"], writes=["convb"])
        for i in range(1):
            S.dve(lambda e, p=pT[i]: e.memset(p[:, 0:1], 0.0), writes=[("pT", i)])
            S.dve(lambda e, p=pT[i]: e.memset(p[:, L + 1:L + 2], 0.0), writes=[("pT", i)])
        load_wchunk(0, 0)
        for cl in range(4):
            conv_chunk(0, cl, cl, x0T[:, cl, :], ("x0T", cl), 0, tmp=u0, tres="u0")
        DUMP("x0T", x0T, [128, 4, L], [("x0T", c) for c in range(4)])
        A.reset(PQ_END)
        Gb.append((A.alloc(8 * 512, BF16).rearrange("p (j t) -> p j t", j=8), A.alloc(8 * 512, BF16).rearrange("p (j t) -> p j t", j=8)))
        Gb.append((A.alloc(8 * 512, BF16).rearrange("p (j t) -> p j t", j=8), A.alloc(8 * 512, BF16).rearrange("p (j t) -> p j t", j=8)))
        sq_t = [A.alloc(512, BF16) for _ in range(2)]
        rstdb = A.alloc(L)
        rtmpb = A.alloc(512)
        hynw = A.alloc(4)
        S.dma(hynw, T["hynw"], writes=["hynw"])
        epsb = A.alloc(1)
        S.dve(lambda e: e.memset(epsb, EPS), writes=["epsb"])
        for oc in range(4):
            par, half = oc // 2, oc % 2
            Gcj, Gsj = Gb[oc % 2]
            S.dma(Gcj, T["Gc"][oc], writes=[("Gc", oc % 2)])
            S.dma(Gsj, T["Gs"][oc], writes=[("Gs", oc % 2)])
            so = 0 if par == 0 else 2
            for c in range(4):
                bank = (oc * 4 + c) % 4
                lst = []
                for j in range(8):
                    lst.append((PS[bank][:, :], PQ[:, j, so, c * 128:(c + 1) * 128], Gcj[:, j, :], j == 0, False))
                    lst.append((PS[bank][:, :], PQ[:, j, so + 1, c * 128:(c + 1) * 128], Gsj[:, j, :], False, j == 7))
                MMG(lst, [("PQ", j) for j in range(8)] + [("Gc", oc % 2), ("Gs", oc % 2)], [psr(bank)])
                x0v = bview(x0T, c * L + par + 1024 * half, [[2, 512]])
                zv = bview(yT, c * L + par + 1024 * half, [[2, 512]])
                TT(zv, PS[bank][:, :], x0v, ALU.mult, [psr(bank), ("x0T", c)], [("z", c, oc)])
        DUMP("zT", yT[:, 0:4, :], [128, 4, L], [("z", c, oc) for c in range(4) for oc in range(4)])
        for tcn in range(4):
            zres = [("z", c, oc) for c in range(4) for oc in range(4)]
            lst = []
            for c in range(4):
                sq = sq_t[c % 2]
                TT(sq, yT[:, c, tcn * 512:(tcn + 1) * 512], yT[:, c, tcn * 512:(tcn + 1) * 512], ALU.mult, zres, [("sq", c % 2)], eng="pool")
                MMG([(PS[4 + tcn % 2][:, :], ones_bf, sq, c == 0, c == 3)], [("sq", c % 2), "ones_bf"], [psr(4 + tcn % 2)])
            ACT(rtmpb, PS[4 + tcn % 2][:, :], AF.Ln, [psr(4 + tcn % 2), "epsb"], ["rtmpb"], scale=1.0 / 512, bias=epsb[:, 0:1])
            ACT(rstdb[:, tcn * 512:(tcn + 1) * 512], rtmpb, AF.Exp, ["rtmpb"], [("rstdb", tcn)], scale=-0.5)
            for c in range(4):
                STT(yT[:, c, tcn * 512:(tcn + 1) * 512], yT[:, c, tcn * 512:(tcn + 1) * 512], hynw[:, c:c + 1], rstdb[:, tcn * 512:(tcn + 1) * 512],
                    ALU.mult, ALU.mult, zres + ["hynw", ("rstdb", tcn)], [("yT", "h", c, tcn)])
        DUMP("yTh", yT[:, 0:4, :], [128, 4, L], [("yT", "h", c, t) for c in range(4) for t in range(4)])
        S.barrier()
        A.reset(PERS)
        if stop_after <= 6:
            S.emit(st)
            return nc, dbg_specs, S

        xaccF = A.alloc(16 * 1056).rearrange("p (i d) -> p i d", i=16)
        xacc = xaccF[:, :, 0:1024]
        ext_s = xaccF[:, :, 1024:1056]
        cnt_i = A.alloc(64).bitcast(mybir.dt.int32)
        tok_i = A.alloc(16).bitcast(mybir.dt.int32)
        P4 = A.mark()
        ext_n = ext_s
        wo_st = A.alloc(4 * 1024).rearrange("p (k n) -> p k n", k=4)
        wo = A.alloc(8 * 1024, BF16).rearrange("p (k n) -> p k n", k=8)
        rw = A.alloc(8 * 20).rearrange("p (k n) -> p k n", k=8)
        rw_bf = A.alloc(8 * 20, BF16).rearrange("p (k n) -> p k n", k=8)
        rbb = A.alloc(20)
        junk = A.alloc(1024)
        xsb = [A.alloc(1024, BF16) for _ in range(2)]
        ss = A.alloc(16); rstd = A.alloc(16); rtmp = A.alloc(16)
        rsm = A.alloc(256)
        wo_v = T["w_out"].rearrange("(k p) n -> p k n", p=128)
        for k in range(8):
            S.dma(wo_st[:, k % 4, :], wo_v[:, k, :], writes=[("wo_st", k % 4)])
            TT(wo[:, k, :], wo_st[:, k % 4, :], g1b, ALU.mult, [("wo_st", k % 4)], [("wo", k)], eng="pool")
        for i in range(16):
            S.dma(xacc[:, i, :], x_v[:, i, :], writes=[("xacc", i)])
        S.dma(rw, T["rw"].rearrange("(k p) n -> p k n", p=128), writes=["rw"])
        S.dma(rbb, bass.AP(T["rb"].tensor, 0, [[0, 128], [1, 20]]), writes=["rbb"])
        CP(rw_bf, rw, ["rw"], ["rw_bf"])
        for i in range(16):
            for dh in range(2):
                bank = (i * 2 + dh) % 4
                lst = [(PS[bank][:, :], yT[:, k, i * 128:(i + 1) * 128], wo[:, k, dh * 512:(dh + 1) * 512], k == 0, k == 7) for k in range(8)]
                MMG(lst, [("wo", k) for k in range(8)], [psr(bank)])
                TT(xacc[:, i, dh * 512:(dh + 1) * 512], PS[bank][:, :], xacc[:, i, dh * 512:(dh + 1) * 512], ALU.add, [psr(bank), ("xacc", i)], [("xacc", i)])
        DUMP("x1", xacc, [128, 16, 1024], [("xacc", i) for i in range(16)])

        def x1_tile(i):
            return xacc[:, i, :], [("xacc", i)]
        norm_to_T(x1_tile, 16, s2, b2, hT, "h2", ["s2", "b2"], junk, xsb, ss, rstd, rtmp)
        DUMP("h2T", hT, [128, 8, L], [("h2T", k, g) for k in range(8) for g in range(4)])
        ra = lambda n_: A.alloc(n_)
        lgA = ra(320).rearrange("p (i c) -> p i c", c=20)
        le_c = ra(256); mxA = ra(16); ohA = ra(64); d4 = ra(64); smA = ra(16); pselA = ra(16)
        tmA = ra(256); le4 = ra(64); m1A = ra(16); mskA = ra(64); le4b = ra(64); m2A = ra(16); selA = ra(64)
        e4A = ra(64); s4A = ra(16); rpA = ra(16); pwA = ra(64)
        v3 = lambda ap: ap.rearrange("p (i c) -> p i c", i=16)
        bc4 = lambda ap: bview(ap, 0, [[1, 16], [0, 4]])
        for i in range(16):
            lst = [(PS[4][:, i * 20:(i + 1) * 20], hT[:, k, i * 128:(i + 1) * 128], rw_bf[:, k, :], k == 0, k == 7) for k in range(8)]
            MMG(lst, [("h2T", k, i // 4) for k in range(8)] + ["rw_bf"], [psr(4)])
        R = ["rt"]
        TT(lgA, PS[4][:, 0:320].rearrange("p (i c) -> p i c", c=20), bview(rbb, 0, [[0, 16], [1, 20]]), ALU.add, [psr(4), "rbb"], R)
        S.dve(lambda e: e.tensor_reduce(out=mxA, in_=lgA[:, :, 0:4], axis=AX.X, op=ALU.max), R, R)
        TT(v3(ohA), lgA[:, :, 0:4], bc4(mxA), ALU.is_ge, R, R)
        TT(v3(d4), lgA[:, :, 0:4], bc4(mxA), ALU.subtract, R, R)
        ACT(d4, d4, AF.Exp, R, R)
        S.dve(lambda e: e.tensor_reduce(out=smA, in_=v3(d4), axis=AX.X, op=ALU.add), R, R)
        S.dve(lambda e: e.reciprocal(out=pselA, in_=smA), R, R)
        CP(v3(le_c), lgA[:, :, 4:20], R, R)
        TT(tmA.rearrange("p (q e) -> p q e", e=4), le_c.rearrange("p (q e) -> p q e", e=4), bview(ohA, 0, [[1, 64], [0, 4]]), ALU.mult, R, R)
        tm3 = v3(tmA)
        TT(v3(le4), tm3[:, :, 0:4], tm3[:, :, 4:8], ALU.add, R, R)
        TT(v3(le4), v3(le4), tm3[:, :, 8:12], ALU.add, R, R)
        TT(v3(le4), v3(le4), tm3[:, :, 12:16], ALU.add, R, R)
        S.dve(lambda e: e.tensor_reduce(out=m1A, in_=v3(le4), axis=AX.X, op=ALU.max), R, R)
        TT(v3(mskA), v3(le4), bc4(m1A), ALU.is_ge, R, R)
        STT(le4b, mskA, -1e30, le4, ALU.mult, ALU.add, R, R)
        S.dve(lambda e: e.tensor_reduce(out=m2A, in_=v3(le4b), axis=AX.X, op=ALU.max), R, R)
        TT(v3(selA), v3(le4), bc4(m2A), ALU.is_ge, R, R)
        TT(v3(e4A), v3(le4), bc4(m1A), ALU.subtract, R, R)
        ACT(e4A, e4A, AF.Exp, R, R)
        TT(e4A, e4A, selA, ALU.mult, R, R)
        S.dve(lambda e: e.tensor_reduce(out=s4A, in_=v3(e4A), axis=AX.X, op=ALU.add), R, R)
        S.dve(lambda e: e.reciprocal(out=rpA, in_=s4A), R, R)
        TT(rpA, rpA, pselA, ALU.mult, R, R)
        TT(v3(pwA), v3(e4A), bc4(rpA), ALU.mult, R, R)
        for g in range(4):
            TT(ext_n[:, :, g * 4:(g + 1) * 4], v3(pwA), bview(ohA, g, [[4, 16], [0, 4]]), ALU.mult, R, R + [("ext", g)])
        CP(ext_n[:, :, 16:20], v3(ohA), R, R + [("ext", 4)])
        EXT = [("ext", i) for i in range(16)]
        tokid_t = A.alloc(16)
        S.dma(tokid_t, T["tokid"], writes=["tokid_t"])
        CP(ext_n[:, :, 20], tokid_t, ["tokid_t"], ["ext_tok"])
        S.dve(lambda e: e.memset(ext_n[:, :, 21:32], 0.0), writes=["ext_pad"])
        EXT = EXT + ["ext_tok", "ext_pad"]
        DUMP("comb", ext_n[:, :, 0:16], [128, 16, 16], EXT)
        tri_f = A.alloc(128); tri_b = A.alloc(128, BF16)
        oh_bf = A.alloc(64, BF16).rearrange("p (i g) -> p i g", g=4)
        cnt_sb = A.alloc(64).rearrange("p (i g) -> p i g", g=4)
        tp = A.alloc(64).rearrange("p (i g) -> p i g", g=4)
        off = A.alloc(64).rearrange("p (i g) -> p i g", g=4)
        tot = A.alloc(4); gbase = A.alloc(4); gend = A.alloc(4)
        posf = A.alloc(16)
        pos_i = A.alloc(16).bitcast(mybir.dt.int32)
        i128 = A.alloc(64).rearrange("p (i g) -> p i g", g=4)
        lo_t = A.alloc(64).rearrange("p (i g) -> p i g", g=4)
        hi_t = A.alloc(64).rearrange("p (i g) -> p i g", g=4)
        S.dma(i128, T["i128"].rearrange("p (i g) -> p i g", g=4), writes=["i128"])
        S.pool(lambda e: e.affine_select(out=tri_f, in_=ones_f, pattern=[[1, 128]], compare_op=ALU.is_ge,
                                         fill=0.0, base=-1, channel_multiplier=-1), writes=["tri_f"])
        CP(tri_b, tri_f, ["tri_f"], ["tri_b"])
        CP(oh_bf, ext_n[:, :, 16:20], EXT, ["oh_bf"])
        MMG([(PS[6][:, i * 4:(i + 1) * 4], tri_b, oh_bf[:, i, :], True, True) for i in range(16)], ["tri_b", "oh_bf"], [psr(6)])
        MMG([(PS[7][:, i * 4:(i + 1) * 4], ones_bf, oh_bf[:, i, :], True, True) for i in range(16)], ["oh_bf"], [psr(7)])
        CP(cnt_sb, PS[7][:, 0:64].rearrange("p (i g) -> p i g", g=4), [psr(7)], ["cnt_sb"])
        S.dve(lambda e: e.memset(tp[:, 0, :], 0.0), writes=["tp"])
        for i in range(1, 16):
            TT(tp[:, i, :], tp[:, i - 1, :], cnt_sb[:, i - 1, :], ALU.add, ["tp", "cnt_sb"], ["tp"])
        TT(tot, tp[:, 15, :], cnt_sb[:, 15, :], ALU.add, ["tp", "cnt_sb"], ["tot"])
        S.dve(lambda e: e.memset(gbase[:, 0:1], 0.0), writes=["gbase"])
        for g in range(1, 4):
            TT(gbase[:, g:g + 1], gbase[:, g - 1:g], tot[:, g - 1:g], ALU.add, ["gbase", "tot"], ["gbase"])
        TT(gend, gbase, tot, ALU.add, ["gbase", "tot"], ["gend"])
        TT(off, PS[6][:, 0:64].rearrange("p (i g) -> p i g", g=4), tp, ALU.add, [psr(6), "tp"], ["off"])
        TT(off, off, bview(gbase, 0, [[0, 16], [1, 4]]), ALU.add, ["off", "gbase"], ["off"])
        TT(off, off, ext_n[:, :, 16:20], ALU.mult, ["off"] + EXT, ["off"])
        S.dve(lambda e: e.tensor_reduce(out=posf, in_=off, axis=AX.X, op=ALU.add), ["off"], ["posf"])
        CP(pos_i, posf, ["posf"], ["pos_i"])
        TT(lo_t, i128, bview(gbase, 0, [[0, 16], [1, 4]]), ALU.max, ["i128", "gbase"], ["lo_t"])
        TS(hi_t, i128, 128.0, None, ALU.add, None, ["i128"], ["hi_t"])
        TT(hi_t, hi_t, bview(gend, 0, [[0, 16], [1, 4]]), ALU.min, ["hi_t", "gend"], ["hi_t"])
        TT(hi_t, hi_t, lo_t, ALU.subtract, ["hi_t", "lo_t"], ["hi_t"])
        TS(hi_t, hi_t, 0.0, None, ALU.max, None, ["hi_t"], ["hi_t"])
        CP(cnt_i, hi_t.rearrange("p i g -> p (i g)"), ["hi_t"], ["cnt_i"])
        DUMP("posf", posf, [128, 16], ["posf"])
        scr_x = nc.dram_tensor("scr_x", [L, 1056], F32, kind="Internal").ap()
        for i in range(16):
            S.add("pool", lambda e, i=i: e.indirect_dma_start(out=scr_x[:, :], out_offset=bass.IndirectOffsetOnAxis(ap=pos_i[:, i:i + 1], axis=0),
                                                              in_=xaccF[:, i, :], in_offset=None),
                  [("xacc", i), "pos_i"] + EXT, [("scr_x", i)], dma=True)
        SCR = [("scr_x", i) for i in range(16)]
        sx_v = scr_x.rearrange("(i p) d -> p i d", p=128)
        for i in range(16):
            S.dma(xaccF[:, i, :], sx_v[:, i, :], reads=SCR, writes=[("xacc", i), ("ext_s", i)] + (EXT if i == 15 else []))
        CP(tok_i, ext_s[:, :, 20], [("ext_s", i) for i in range(16)], ["tok_i"])
        DUMP("x1s", xacc, [128, 16, 1024], [("xacc", i) for i in range(16)])
        DUMP("ext_s", ext_s, [128, 16, 32], [("ext_s", i) for i in range(16)])
        norm_to_T(x1_tile, 16, s2, b2, hT, "h2", ["s2", "b2"], junk, xsb, ss, rstd, rtmp)
        S.barrier()
        A.reset(P4)
        if stop_after <= 7:
            S.emit(st)
            return nc, dbg_specs, S

        w1b = [A.alloc(8 * 512, BF16).rearrange("p (k n) -> p k n", k=8) for _ in range(2)]
        w3b = [A.alloc(8 * 512, BF16).rearrange("p (k n) -> p k n", k=8) for _ in range(2)]
        w2st = A.alloc(4 * 1024).rearrange("p (k n) -> p k n", k=4)
        sa_t = [A.alloc(512) for _ in range(2)]
        hid_t = [A.alloc(512, BF16) for _ in range(2)]
        hidT_t = [A.alloc(512, BF16).rearrange("p (k t) -> p k t", k=4) for _ in range(2)]
        yflat = yT.rearrange("p k t -> p (k t)")
        w2b = [yflat[:, i * 4096:(i + 1) * 4096].rearrange("p (k n) -> p k n", k=4) for i in range(2)]
        w1_v = T["exp_w1"].rearrange("e (k p) n -> e p k n", p=128)
        w3_v = T["exp_w3"].rearrange("e (k p) n -> e p k n", p=128)
        w2_v = T["exp_w2"].rearrange("e (k p) n -> e p k n", p=128)

        def load_expert(e):
            s = e % 2
            for hk in range(2):
                S.dma(w1b[s][:, hk * 4:(hk + 1) * 4, :], w1_v[e][:, hk * 4:(hk + 1) * 4, :], writes=[("w1", s, hk)], eng="pool")
            for hk in range(2):
                S.dma(w3b[s][:, hk * 4:(hk + 1) * 4, :], w3_v[e][:, hk * 4:(hk + 1) * 4, :], writes=[("w3", s, hk)], eng="pool")
            S.dma(w2st, w2_v[e], writes=["w2st"])
            for k in range(4):
                TT(w2b[s][:, k, :], w2st[:, k, :], g2b, ALU.mult, ["w2st"], [("w2", s)], eng="pool")

        blocks = [(e, i) for e in range(16) for i in range(16)]

        def stage1(b):
            e, i = blocks[b]
            s, g, par = e % 2, e // 4, b % 2
            pa, pu = par, 2 + par
            if i == 2 and e + 1 < 16:
                load_expert(e + 1)
            if i == 0 and e % 4 == 0:
                for en in S.cond_engs:
                    S.regload(en, "g%d" % (g % 2), bview(cnt_i[0:1, :], g, [[4, 16]]), 16)
            S.cond_begin(("g%d" % (g % 2), i))
            lst = [(PS[pa][:, :], hT[:, k, i * 128:(i + 1) * 128], w1b[s][:, k, :], k == 0, k == 7) for k in range(8)]
            MMG(lst, [("w1", s, 0), ("w1", s, 1)], [psr(pa)])
            lst = [(PS[pu][:, :], hT[:, k, i * 128:(i + 1) * 128], w3b[s][:, k, :], k == 0, k == 7) for k in range(8)]
            MMG(lst, [("w3", s, 0), ("w3", s, 1)], [psr(pu)])
            ACT(sa_t[par], PS[pa][:, :], AF.Silu, [psr(pa)], [("sa", par)])
            STT(hid_t[par], PS[pu][:, :], ext_s[:, i, e:e + 1], sa_t[par], ALU.mult, ALU.mult, [psr(pu), ("sa", par)], [("hid", par)])
            S.cond_end()

        def stage2(b):
            e, i = blocks[b]
            par = b % 2
            S.cond_begin(("g%d" % ((e // 4) % 2), i))
            lst = [(PSB[4][:, par * 512 + hc * 128:par * 512 + (hc + 1) * 128], hid_t[par][:, hc * 128:(hc + 1) * 128], ident) for hc in range(4)]
            TRG(lst, [("hid", par)], [psr(4)])
            ACT(hidT_t[par], PSB[4][:, par * 512:(par + 1) * 512].rearrange("p (k t) -> p k t", k=4), AF.Copy, [psr(4)], [("hidT", par)])
            S.cond_end()

        def stage3(b):
            e, i = blocks[b]
            s, g, par = e % 2, e // 4, b % 2
            py0, py1 = 5, 6
            S.cond_begin(("g%d" % (g % 2), i))
            for dh, py in ((0, py0), (1, py1)):
                lst = [(PS[py][:, :], hidT_t[par][:, k, :], w2b[s][:, k, dh * 512:(dh + 1) * 512], k == 0, k == 3) for k in range(4)]
                MMG(lst, [("hidT", par), ("w2", s)], [psr(py)])
            for dh, py in ((0, py0), (1, py1)):
                STT(xacc[:, i, dh * 512:(dh + 1) * 512], PS[py][:, :], ext_s[:, i, 16 + g:17 + g], xacc[:, i, dh * 512:(dh + 1) * 512],
                    ALU.mult, ALU.add, [psr(py), ("xacc", i)], [("xacc", i)])
            S.cond_end()

        dummy = A.alloc(1200)
        dcnt = [0]

        def dcol():
            dcnt[0] += 1
            return dummy[0:1, dcnt[0] - 1:dcnt[0]]
        S.else_fn = {"pe": lambda eo: eo.matmul(PS[7][0:1, 0:1], lhsT=ident[0:1, 0:1], rhs=ident[0:1, 0:1], start=True, stop=True),
                     "dve": lambda eo: eo.memset(dcol(), 0.0),
                     "act": lambda eo: eo.activation(out=dcol(), in_=ones_f[0:1, 0:1], func=AF.Copy)}
        S.cond_engs = ("pe", "dve")
        load_expert(0)
        NB = len(blocks)
        for step in range(NB + 2):
            if step < NB:
                stage1(step)
            if 0 <= step - 1 < NB:
                stage2(step - 1)
            if 0 <= step - 2 < NB:
                stage3(step - 2)
        S.cond_engs = ("pe",)
        DUMP("x2", xacc, [128, 16, 1024], [("xacc", i) for i in range(16)])

        ssf = A.alloc(16); rstdf = A.alloc(16); rtmpf = A.alloc(16)
        junkf = sa_t[0]
        for i in range(16):
            for hh in range(2):
                ACT(junkf, xacc[:, i, hh * 512:(hh + 1) * 512], AF.Square, [("xacc", i)], [("sa", 0), ("ssf", i, hh)], accum_out=rtmpf[:, i:i + 1] if hh else ssf[:, i:i + 1])
            TT(ssf[:, i:i + 1], ssf[:, i:i + 1], rtmpf[:, i:i + 1], ALU.add, [("ssf", i, 0), ("ssf", i, 1)], [("ssf", i)])
            TS(ssf[:, i:i + 1], ssf[:, i:i + 1], 1.0 / D, EPS, ALU.mult, ALU.add, [("ssf", i)], [("ssf", i)])
            ACT(ssf[:, i:i + 1], ssf[:, i:i + 1], AF.Sqrt, [("ssf", i)], [("ssf", i)])
            S.dve(lambda e, i=i: e.reciprocal(out=rstdf[:, i:i + 1], in_=ssf[:, i:i + 1]), [("ssf", i)], [("rstdf", i)])
            STT(xacc[:, i, :], xacc[:, i, :], rstdf[:, i:i + 1], fnwb, ALU.mult, ALU.mult, [("xacc", i), ("rstdf", i), "fnwb"], [("xacc", i)])
            S.add("pool", lambda e, i=i: e.indirect_dma_start(out=out_d[:, :], out_offset=bass.IndirectOffsetOnAxis(ap=tok_i[:, i:i + 1], axis=0),
                                                              in_=xacc[:, i, :], in_offset=None),
                  [("xacc", i)], [("out", i)], dma=True)
        S.emit(st)
    return nc, dbg_specs, S


def make_in_maps(inp):
    f = lambda a: np.ascontiguousarray(np.asarray(a, dtype=np.float32))
    C = host_constants()
    fm = lambda v, nch: np.ascontiguousarray(f(v).reshape(nch, 128).T)
    shared = {
        "ada_w": f(inp["ada_w"][0]), "ada_b": f(inp["ada_b"][0]), "adab_fm": fm(inp["ada_b"][0], 48),
        "nw1": fm(inp["norm1_w"][0], 8), "nw2": fm(inp["norm2_w"][0], 8), "fnw": f(inp["final_norm_w"]),
        "w_in": f(inp["w_in"][0]),
        "convw": np.ascontiguousarray(f(inp["hy_conv_w"][0]).reshape(3, 12, 128).transpose(2, 1, 0)),
        "convb": fm(inp["hy_conv_b"][0], 12),
        "f_w1": f(inp["hy_f_w1"][0]), "f_w2": f(inp["hy_f_w2"][0]), "f_w3": f(inp["hy_f_w3"][0]), "f_wout": f(inp["hy_f_wout"][0]),
        "f_b": np.ascontiguousarray(np.stack([f(inp["hy_f_b1"][0]), f(inp["hy_f_b2"][0]), f(inp["hy_f_b3"][0])], 1)),
        "f_freq": f(inp["hy_f_freq"][0]).reshape(64, 1),
        "hy_skip": f(inp["hy_skip"][0]).reshape(1, 512), "hynw": fm(inp["hy_out_norm"][0], 4), "gnw": f(inp["ret_gn_w"][0]),
        "w_out": f(inp["w_out"][0]),
        "rw": np.ascontiguousarray(np.concatenate([f(inp["router_g_w"][0]), f(inp["router_e_w"][0])], 1)),
        "rb": np.ascontiguousarray(np.concatenate([f(inp["router_g_b"][0]), f(inp["router_e_b"][0])], 0)),
        "exp_w1": f(inp["exp_w1"][0]).reshape(16, D, 512), "exp_w3": f(inp["exp_w3"][0]).reshape(16, D, 512),
        "exp_w2": f(inp["exp_w2"][0]).reshape(16, 512, D),
    }
    for name, shape, dt in CONST_SPECS:
        shared["k_" + name] = C[name]
    x = f(inp["x"]); ctx = f(inp["ctx"]); c = f(inp["c"]); c_ctx = f(inp["c_ctx"])
    maps = []
    for b in range(8):
        m = dict(shared)
        m["x"] = x[b]
        m["ctx"] = ctx[b]
        m["cc"] = np.ascontiguousarray(np.stack([c[b].reshape(8, 128).T, c_ctx.reshape(8, 128).T], 2))
        maps.append(m)
    return maps


_NC = None


def kernel(**inputs):
    global _NC
    if _NC is None:
        _NC = build()[0]
    maps = make_in_maps(inputs)
    res = run_bass_kernel_spmd(_NC, maps, core_ids=list(range(8)))
    return np.stack([np.asarray(r["out"], dtype=np.float32) for r in res.results], 0)
```

```python
import contextlib
import math
import numpy as np
import ml_dtypes
import concourse.bass as bass
import concourse.mybir as mybir
from concourse.bass_utils import run_bass_kernel_spmd

F32 = mybir.dt.float32
BF16 = mybir.dt.bfloat16
AF = mybir.ActivationFunctionType
ALU = mybir.AluOpType
AX = mybir.AxisListType

D = 1024
L = 2048
NT = 16
CTX = 256
EPS = 1e-6
MAGIC = 12582912.0
TWO_PI = 2.0 * math.pi
NFFT = 4096


class Sched:
    def __init__(self, nc, n_dma_sems=12):
        self.nc = nc
        self.ops = []
        self.last_w = {}
        self.readers = {}
        self.n_dma_sems = n_dma_sems
        self.fence = set()
        self.since_fence = {}
        self.cur_cond = None
        self.else_fn = {}
        self.cond_engs = ("pe",)
        self.n_cond = 0
        self.cond_src = {}

    def add(self, eng, fn, reads=(), writes=(), dma=False):
        oid = len(self.ops)
        deps = set(self.fence)
        for r in reads:
            if r in self.last_w:
                deps.add(self.last_w[r])
            if isinstance(r, tuple) and r[0] == "ps":
                deps.update(o for o in self.readers.get(r, ()) if self.ops[o]["eng"] != eng)
        for r in writes:
            if r in self.last_w:
                deps.add(self.last_w[r])
            deps.update(self.readers.get(r, ()))
        self.ops.append(dict(eng=eng, fn=fn, deps=deps, dma=dma, cond=(self.cur_cond if (eng in self.cond_engs and not dma) else None)))
        for r in reads:
            self.readers.setdefault(r, set()).add(oid)
        for r in writes:
            self.last_w[r] = oid
            self.readers[r] = set()
        if dma:
            self.since_fence[("dma", oid)] = oid
        else:
            self.since_fence[eng] = oid
        return oid

    def cond_begin(self, src_ap):
        self.n_cond += 1
        self.cur_cond = self.n_cond
        self.cond_src[self.cur_cond] = src_ap

    def cond_end(self):
        self.cur_cond = None

    def regload(self, eng, key, ap, n):
        def fn(eo):
            regs = self._regbank(eo, (eng, key), n)
            return eo.reg_load(regs, ap)
        return self.add(eng, fn, (), ())

    def _regbank(self, eo, key, n):
        if key not in self._regs:
            self._regs[key] = [self._stack.enter_context(eo.register(f"cr_{key[0]}_{key[1]}_{j}")) for j in range(n)]
        return self._regs[key]

    def barrier(self):
        self.fence = set(self.since_fence.values())
        self.since_fence = {}
        self.last_w = {}
        self.readers = {}

    def pe(self, fn, reads=(), writes=()):
        return self.add("pe", fn, reads, writes)

    def act(self, fn, reads=(), writes=()):
        return self.add("act", fn, reads, writes)

    def dve(self, fn, reads=(), writes=()):
        return self.add("dve", fn, reads, writes)

    def pool(self, fn, reads=(), writes=()):
        return self.add("pool", fn, reads, writes)

    def dma(self, out, in_, reads=(), writes=(), eng="sp", **kw):
        return self.add(eng, lambda e: e.dma_start(out=out, in_=in_, **kw), reads, writes, dma=True)

    def emit(self, stack):
        nc = self.nc
        self._stack = stack
        self._regs = {}
        ops = self.ops
        engs = ["pe", "act", "dve", "pool", "sp"]
        esem = {e: stack.enter_context(nc.semaphore("sem_" + e)) for e in engs}
        dsem = {e: [stack.enter_context(nc.semaphore(f"dsem_{e}{i}")) for i in range(self.n_dma_sems)]
                for e in ("sp", "pool", "act")}

        def skip(pd, o):
            return (not pd["dma"]) and pd["eng"] == o["eng"] and (not o["dma"]) and o["eng"] == "pe"

        target = [False] * len(ops)
        for o in ops:
            for d in o["deps"]:
                pd = ops[d]
                if pd["dma"] or skip(pd, o):
                    continue
                target[d] = True
        cnt = {e: 0 for e in engs}
        dcnt = {e: 0 for e in dsem}
        dval = {e: [0] * self.n_dma_sems for e in dsem}
        token = {}
        prewait = {}
        for i, o in enumerate(ops):
            e = o["eng"]
            if o["dma"]:
                k = dcnt[e] % self.n_dma_sems
                dcnt[e] += 1
                prev = dval[e][k]
                if prev:
                    prewait[i] = (dsem[e][k], prev)
                dval[e][k] = prev + 16
                token[i] = (dsem[e][k], prev + 16)
            elif target[i]:
                cnt[e] += 1
                token[i] = (esem[e], cnt[e])
        per_eng = {e: [] for e in engs}
        seen = {e: {} for e in engs}
        cond_seen = {e: None for e in engs}
        cond_cur = {e: None for e in engs}
        for i, o in enumerate(ops):
            e = o["eng"]
            need = {}
            if o["cond"] != cond_cur[e]:
                cond_cur[e] = o["cond"]
                cond_seen[e] = dict(seen[e]) if cond_cur[e] is not None else None
            seen_e = cond_seen[e] if cond_cur[e] is not None else seen[e]

            def want(s, v):
                key = id(s)
                if need.get(key, (None, 0))[1] < v:
                    need[key] = (s, v)

            for d in o["deps"]:
                pd = ops[d]
                if skip(pd, o):
                    continue
                want(*token[d])
            if i in prewait:
                want(*prewait[i])
            waits = []
            for key, (s, v) in need.items():
                if seen_e.get(key, 0) >= v:
                    continue
                seen_e[key] = v
                waits.append((s, v))
            per_eng[e].append((o, waits, token.get(i)))
        final_waits = []
        for e in dsem:
            for k in range(self.n_dma_sems):
                if dval[e][k]:
                    final_waits.append((dsem[e][k], dval[e][k]))
        self.stats = {e: len(per_eng[e]) for e in engs}
        self.nwaits = {e: sum(len(w) for _, w, _ in per_eng[e]) for e in engs}

        def emit_one(eo, o, waits, tok):
            for s, v in waits:
                eo.wait_ge(s, v)
            inst = o["fn"](eo)
            if tok is not None:
                inst.then_inc(tok[0], 16 if o["dma"] else 1)

        def run(e, eo):
            lst = per_eng[e]
            idx = 0
            creg = None
            while idx < len(lst):
                o, waits, tok = lst[idx]
                if o["cond"] is None:
                    emit_one(eo, o, waits, tok)
                    idx += 1
                    continue
                c = o["cond"]
                j = idx
                while j < len(lst) and lst[j][0]["cond"] == c:
                    j += 1
                ntok = sum(1 for k in range(idx, j) if lst[k][2] is not None)
                src = self.cond_src[c]
                if isinstance(src, tuple):
                    breg = self._regs[(e, src[0])][src[1]]
                else:
                    if creg is None:
                        creg = stack.enter_context(eo.register("condreg_" + e))
                    eo.reg_load(creg, src)
                    breg = creg
                with eo.If_ne(breg, 0):
                    for k in range(idx, j):
                        emit_one(eo, *lst[k])
                if ntok:
                    with eo.Else():
                        if e in self.else_fn:
                            self.else_fn[e](eo).then_inc(esem[e], ntok)
                        else:
                            eo.drain().then_inc(esem[e], ntok)
                idx = j
            if e == "sp":
                for s, v in final_waits:
                    eo.wait_ge(s, v)
                for ee in engs:
                    if ee != "sp" and cnt[ee]:
                        eo.wait_ge(esem[ee], cnt[ee])

        with nc.Block() as block:
            @block.tensor
            def _(eo):
                run("pe", eo)

            @block.scalar
            def _(eo):
                run("act", eo)

            @block.vector
            def _(eo):
                run("dve", eo)

            @block.gpsimd
            def _(eo):
                run("pool", eo)

            @block.sync
            def _(eo):
                run("sp", eo)


class Carver:
    def __init__(self, big, nwords):
        self.big = big
        self.n = nwords
        self.top = 0
        self.peak = 0

    def alloc(self, nelem, dt=F32):
        words = nelem if dt == F32 else (nelem + 1) // 2
        o = self.top
        self.top += words
        self.peak = max(self.peak, self.top)
        assert self.top <= self.n, f"SBUF carve overflow {self.top} > {self.n}"
        v = self.big[:, o:o + words]
        return v if dt == F32 else v.bitcast(dt)

    def mark(self):
        return self.top

    def reset(self, m):
        self.top = m


def bview(ap, off, pat):
    return bass.AP(ap.tensor, ap.offset + off, [list(ap.ap[0])] + [list(p) for p in pat])


_CONST = None


def _bf(a):
    return np.ascontiguousarray(a.astype(ml_dtypes.bfloat16))


def host_constants():
    global _CONST
    if _CONST is not None:
        return _CONST
    c = {}
    t = np.linspace(0.0, 1.0, L, dtype=np.float32)[:, None]
    bands = np.linspace(1e-4, 15, 16, dtype=np.float32)
    ang = (np.float32(2.0 * math.pi / L) * np.arange(L, dtype=np.float32)[:, None]) * bands[None, :]
    z = np.concatenate([t, np.cos(ang), -np.sin(ang)], axis=-1).astype(np.float32)
    c["zT"] = np.ascontiguousarray(z.T)
    max_decay = math.log(1e-2) / 0.3
    min_decay = math.log(1e-2) / 1.5
    deltas = np.abs(np.linspace(min_decay, max_decay, 512, dtype=np.float32))
    window = (np.exp(-t * deltas[None, :]) + 0.05).astype(np.float32)
    tok = np.zeros((16, 128), dtype=np.int64)
    for par in range(2):
        for jb in range(8):
            tok[par * 8 + jb] = 2 * (128 * jb + np.arange(128)) + par
    c["tok16"] = tok
    wF = window[tok]
    wB = wF.copy()
    wB[0, 0, :] = 0.0
    c["winF"] = np.ascontiguousarray(wF)
    c["winB"] = np.ascontiguousarray(wB)
    pf = np.arange(128)
    Fc = np.zeros((8, 128, 16, 128), dtype=np.float32)
    Fs = np.zeros((8, 128, 16, 128), dtype=np.float32)
    for j in range(8):
        phi = 2.0 * np.pi * (128 * j + pf + 0.5) / NFFT
        a = tok.T[:, :, None].astype(np.float64) * phi[None, None, :]
        Fc[j] = np.cos(a)
        Fs[j] = np.sin(a)
    c["Fc"] = _bf(Fc)
    c["Fs"] = _bf(Fs)
    Gc = np.zeros((4, 128, 8, 512), dtype=np.float32)
    Gs = np.zeros((4, 128, 8, 512), dtype=np.float32)
    for oc in range(4):
        par, half = oc // 2, oc % 2
        tt = 2 * (512 * half + np.arange(512)) + par
        for j in range(8):
            phi = 2.0 * np.pi * (128 * j + pf + 0.5) / NFFT
            a = phi[:, None] * tt[None, :].astype(np.float64)
            Gc[oc, :, j, :] = (2.0 / NFFT) * np.cos(a)
            Gs[oc, :, j, :] = -(2.0 / NFFT) * np.sin(a)
    c["Gc"] = _bf(Gc)
    c["Gs"] = _bf(Gs)
    freqs = (10000.0 ** (-np.arange(16, dtype=np.float32) / 16)).astype(np.float32)
    tn = (128 * np.arange(16)[None, :] + np.arange(128)[:, None])
    rows = (tn // 64).astype(np.float32)
    cols = (tn % 64).astype(np.float32)
    ar = rows[:, :, None] * freqs[None, None, :]
    ac = cols[:, :, None] * freqs[None, None, :]
    c["ropeC"] = np.ascontiguousarray(np.concatenate([np.cos(ar), np.cos(ar), np.cos(ac), np.cos(ac)], -1).astype(np.float32))
    c["ropeS"] = np.ascontiguousarray(np.concatenate([np.sin(ar), np.sin(ar), np.sin(ac), np.sin(ac)], -1).astype(np.float32))
    h = np.arange(4, dtype=np.float64)
    lgf = np.log1p(-np.exp2(-(5.0 + h)))
    lgb = np.log1p(-np.exp2(-(5.5 + h)))
    i = np.arange(128, dtype=np.float64)
    sc = 128.0 ** -0.5
    dif = i[None, :] - i[:, None]
    MT = np.zeros((128, 4, 128))
    for hh in range(4):
        MT[:, hh, :] = (np.where(dif >= 0, np.exp(np.maximum(dif, 0) * lgf[hh]), 0.0)
                        + np.where(dif <= 0, np.exp(np.maximum(-dif, 0) * lgb[hh]), 0.0)) * sc
    c["MT"] = MT.astype(np.float32)
    rep = lambda a: np.ascontiguousarray(np.repeat(a[:, :, None], 128, axis=2).reshape(a.shape[0], 512).astype(np.float32))
    c["zf"] = rep(np.exp((127.0 - i)[:, None] * lgf[None, :]) * sc)
    c["zb"] = rep(np.exp(i[:, None] * lgb[None, :]) * sc)
    c["xi"] = np.ascontiguousarray(np.concatenate([np.exp((i + 1.0)[:, None] * lgf[None, :]),
                                                   np.exp((128.0 - i)[:, None] * lgb[None, :])], 1).astype(np.float32))
    c["gtab"] = np.ascontiguousarray(np.stack([rep(np.tile(np.exp(128.0 * lgf)[None, :], (128, 1))),
                                               rep(np.tile(np.exp(128.0 * lgb)[None, :], (128, 1)))], 1))
    m = np.arange(256, dtype=np.float64)
    cwf = rep(np.exp((255.0 - m)[:, None] * lgf[None, :]) * sc).reshape(2, 128, 512)
    cwb = rep(np.exp(m[:, None] * lgb[None, :]) * sc).reshape(2, 128, 512)
    c["ctxw"] = np.ascontiguousarray(np.stack([cwf, cwb], 1).transpose(2, 0, 1, 3))
    xiT = np.stack([np.exp((i + 1.0)[None, :] * lgf[:, None]), np.exp((128.0 - i)[None, :] * lgb[:, None])], 0)
    c["xiT"] = np.ascontiguousarray(np.tile(xiT[None], (128, 1, 1, 1)).astype(np.float32))
    c["tokid"] = np.ascontiguousarray((128 * np.arange(16)[None, :] + np.arange(128)[:, None]).astype(np.float32))
    c["i128"] = np.ascontiguousarray(np.tile(np.repeat(128.0 * np.arange(16), 4)[None, :], (128, 1)).astype(np.float32))
    _CONST = c
    return c


CONST_SPECS = [
    ("zT", [33, L], F32), ("winF", [16, 128, 512], F32), ("winB", [16, 128, 512], F32),
    ("Fc", [8, 128, 16, 128], BF16), ("Fs", [8, 128, 16, 128], BF16),
    ("Gc", [4, 128, 8, 512], BF16), ("Gs", [4, 128, 8, 512], BF16),
    ("ropeC", [128, 16, 64], F32), ("ropeS", [128, 16, 64], F32),
    ("MT", [128, 4, 128], F32), ("zf", [128, 512], F32), ("zb", [128, 512], F32),
    ("xi", [128, 8], F32), ("gtab", [128, 2, 512], F32), ("ctxw", [128, 2, 2, 512], F32),
    ("tokid", [128, 16], F32), ("i128", [128, 64], F32), ("xiT", [128, 2, 4, 128], F32),
]

INPUT_SPECS = [
    ("x", [L, D]), ("ctx", [CTX, D]), ("cc", [128, 8, 2]), ("ada_w", [D, 6 * D]), ("ada_b", [6 * D]),
    ("adab_fm", [128, 48]), ("nw1", [128, 8]), ("nw2", [128, 8]), ("fnw", [D]),
    ("w_in", [D, 3584]), ("convw", [128, 12, 3]), ("convb", [128, 12]),
    ("f_w1", [33, 64]), ("f_w2", [64, 64]), ("f_w3", [64, 64]), ("f_wout", [64, 1024]),
    ("f_b", [64, 3]), ("f_freq", [64, 1]), ("hy_skip", [1, 512]), ("hynw", [128, 4]), ("gnw", [512]),
    ("w_out", [D, D]), ("rw", [D, 20]), ("rb", [20]),
    ("exp_w1", [16, D, 512]), ("exp_w3", [16, D, 512]), ("exp_w2", [16, 512, D]),
]


def build(debug=None, stop_after=99, dev_cut=None):
    nc = bass.Bass("TRN2", target_bir_lowering=False)
    T = {}
    for name, shape in INPUT_SPECS:
        T[name] = nc.dram_tensor(name, list(shape), F32, kind="ExternalInput").ap()
    for name, shape, dt in CONST_SPECS:
        T[name] = nc.dram_tensor("k_" + name, list(shape), dt, kind="ExternalInput").ap()
    out_d = nc.dram_tensor("out", [L, D], F32, kind="ExternalOutput").ap()
    dbg_specs = {}

    with contextlib.ExitStack() as st:
        S = Sched(nc)
        NW = 53000
        big = st.enter_context(nc.sbuf_tensor("big", [128, NW], F32))
        A = Carver(big, NW)
        PS = [st.enter_context(nc.psum_tensor(f"ps{i}", [128, 512], F32)) for i in range(8)]
        PSB = [p.bitcast(BF16) for p in PS]

        def psr(i):
            return ("ps", i)

        def ACT(out, in_, func, reads, writes, **kw):
            return S.act(lambda e: e.activation(out=out, in_=in_, func=func, **kw), reads, writes)

        def TT(out, in0, in1, op, reads, writes, eng="dve"):
            return S.add(eng, lambda e: e.tensor_tensor(out=out, in0=in0, in1=in1, op=op), reads, writes)

        def TS(out, in0, s1, s2, op0, op1, reads, writes, eng="dve", **kw):
            if op1 is None:
                return S.add(eng, lambda e: e.tensor_scalar(out=out, in0=in0, scalar1=s1, scalar2=None, op0=op0, **kw), reads, writes)
            return S.add(eng, lambda e: e.tensor_scalar(out=out, in0=in0, scalar1=s1, scalar2=s2, op0=op0, op1=op1, **kw), reads, writes)

        def STT(out, in0, scalar, in1, op0, op1, reads, writes):
            return S.dve(lambda e: e.scalar_tensor_tensor(out=out, in0=in0, scalar=scalar, in1=in1, op0=op0, op1=op1), reads, writes)

        def CP(out, in_, reads, writes, eng="dve"):
            return S.add(eng, lambda e: e.tensor_copy(out=out, in_=in_), reads, writes)

        def MMG(lst, reads, writes):
            def fn(e):
                inst = None
                for (o, l, r, s0, s1) in lst:
                    inst = e.matmul(o, lhsT=l, rhs=r, start=s0, stop=s1)
                return inst
            return S.pe(fn, reads, writes)

        def TRG(lst, reads, writes):
            def fn(e):
                inst = None
                for (o, i_, idn) in lst:
                    inst = e.transpose(out=o, in_=i_, identity=idn)
                return inst
            return S.pe(fn, reads, writes)

        def DUMP(name, ap, shape, reads):
            if debug is None or name not in debug:
                return
            dt = ap.dtype
            d = nc.dram_tensor("dbg_" + name, list(shape), dt, kind="ExternalOutput").ap()
            S.dma(d, ap, reads=reads)
            dbg_specs[name] = (list(shape), dt)

        def rsqrt_ops(out, in_, n, scale, reads, writes, tmp):
            TS(tmp, in_, scale, EPS, ALU.mult, ALU.add, reads, [writes[0] + "_t"])
            ACT(tmp, tmp, AF.Sqrt, [writes[0] + "_t"], [writes[0] + "_t"])
            S.dve(lambda e: e.reciprocal(out=out, in_=tmp), [writes[0] + "_t"], writes)

        ident = A.alloc(128, BF16)
        identf = A.alloc(128)
        ones_bf = A.alloc(128, BF16)
        ones_f = A.alloc(128)
        cc_t = A.alloc(16).rearrange("p (k c) -> p k c", c=2)
        sc_f = A.alloc(16).rearrange("p (k c) -> p k c", c=2)
        sc_b = A.alloc(16, BF16).rearrange("p (k c) -> p k c", c=2)
        nw1 = A.alloc(8)
        nw2 = A.alloc(8)
        adab = A.alloc(48)
        modc = A.alloc(32)
        modx = A.alloc(32)
        s1 = A.alloc(8); b1 = A.alloc(8); cs1 = A.alloc(8); cb1 = A.alloc(8); s2 = A.alloc(8); b2 = A.alloc(8)
        g1b = A.alloc(1024)
        g2b = A.alloc(1024)
        fnwb = A.alloc(1024)
        comb = A.alloc(256).rearrange("p (i e) -> p i e", e=16)
        hT = A.alloc(8 * L, BF16).rearrange("p (k t) -> p k t", k=8)
        yT = A.alloc(8 * L, BF16).rearrange("p (k t) -> p k t", k=8)
        PERS = A.mark()

        S.pool(lambda e: e.memset(identf, 0.0), writes=["identf"])
        S.pool(lambda e: e.affine_select(out=identf, in_=identf, pattern=[[-1, 128]], compare_op=ALU.not_equal,
                                         fill=1.0, base=0, channel_multiplier=1), reads=["identf"], writes=["identf"])
        CP(ident, identf, ["identf"], ["ident"])
        S.pool(lambda e: e.memset(ones_f, 1.0), writes=["ones_f"])
        CP(ones_bf, ones_f, ["ones_f"], ["ones_bf"])

        S.dma(cc_t, T["cc"], writes=["cc"])
        S.dma(nw1, T["nw1"], writes=["nw1"])
        S.dma(nw2, T["nw2"], writes=["nw2"])
        S.dma(adab, T["adab_fm"], writes=["adab"])
        S.dma(fnwb, bass.AP(T["fnw"].tensor, 0, [[0, 128], [1, 1024]]), writes=["fnwb"])
        ACT(sc_f, cc_t, AF.Silu, ["cc"], ["sc_f"])
        CP(sc_b, sc_f, ["sc_f"], ["sc_b"])
        adaw = [A.alloc(8 * 1024, BF16).rearrange("p (k n) -> p k n", k=8) for _ in range(2)]
        ada_v = T["ada_w"].rearrange("(k p) n -> p k n", p=128)
        fm_slot = {0: 0, 1: 1, 3: 2, 4: 3}

        def ada_dma(j, buf, bi):
            for hk in range(2):
                S.dma(buf[:, hk * 4:(hk + 1) * 4, :], ada_v[:, hk * 4:(hk + 1) * 4, j * 1024:(j + 1) * 1024],
                      writes=[(("adaw", bi), k) for k in range(hk * 4, hk * 4 + 4)], eng="pool")

        def ada_mm(j, buf, bi, lhsb_, ab):
            rk = [(("adaw", bi), k) for k in range(8)]
            if j in fm_slot:
                sl = fm_slot[j]
                bank = sl % 2
                lst = []
                for m in range(8):
                    for k in range(8):
                        lst.append((PS[bank][:, m * 2:(m + 1) * 2], buf[:, k, m * 128:(m + 1) * 128], sc_b[:, k, :], k == 0, k == 7))
                MMG(lst, rk + ["sc_b"], [psr(bank)])
                pv = PS[bank][:, 0:16].rearrange("p (m c) -> p m c", c=2)
                TT(modc[:, sl * 8:(sl + 1) * 8], pv[:, :, 0], adab[:, j * 8:(j + 1) * 8], ALU.add, [psr(bank), "adab"], [("modc", sl)])
                TT(modx[:, sl * 8:(sl + 1) * 8], pv[:, :, 1], adab[:, j * 8:(j + 1) * 8], ALU.add, [psr(bank), "adab"], [("modx", sl)])
            else:
                gb = g1b if j == 2 else g2b
                gname = "g1b" if j == 2 else "g2b"
                S.dma(ab, bass.AP(T["ada_b"].tensor, j * 1024, [[0, 128], [1, 1024]]), writes=[gname + "_ab"])
                for half in range(2):
                    bank = 2 + half
                    lst = [(PS[bank][:, :], lhsb_[:, k, :], buf[:, k, half * 512:(half + 1) * 512], k == 0, k == 7) for k in range(8)]
                    MMG(lst, rk + [("lhsb", k) for k in range(8)], [psr(bank)])
                    TT(gb[:, half * 512:(half + 1) * 512], PS[bank][:, :], ab[:, half * 512:(half + 1) * 512], ALU.add,
                       [psr(bank), gname + "_ab"], [(gname, half)])

        ada_dma(0, adaw[0], 0)
        ada_dma(1, adaw[1], 1)
        ada_mm(0, adaw[0], 0, None, None)
        ada_mm(1, adaw[1], 1, None, None)
        STT(s1, modc[:, 8:16], 1.0, nw1, ALU.add, ALU.mult, [("modc", 1), "nw1"], ["s1"])
        CP(b1, modc[:, 0:8], [("modc", 0)], ["b1"])
        STT(cs1, modx[:, 8:16], 1.0, nw1, ALU.add, ALU.mult, [("modx", 1), "nw1"], ["cs1"])
        CP(cb1, modx[:, 0:8], [("modx", 0)], ["cb1"])
        DUMP("s1", s1, [128, 8], ["s1"])
        DUMP("b1", b1, [128, 8], ["b1"])
        S.barrier()
        A.reset(PERS)
        if stop_after <= 0:
            S.emit(st)
            return nc, dbg_specs, S

        def norm_to_T(src_tile, ntiles, sc_ap, bi_ap, dstT, tag, sc_res, junk, xsb, ss, rstd, rtmp):
            ngrp = (ntiles + 3) // 4
            tiles_of = lambda g: list(range(g * 4, min(ntiles, g * 4 + 4)))

            def st_a(g):
                tiles = tiles_of(g)
                for i in tiles:
                    ap, rd = src_tile(i)
                    ACT(junk, ap, AF.Square, rd, [tag + "junk", (tag + "ss", i)], accum_out=ss[:, i:i + 1])
                g0 = tiles[0]
                rsqrt_ops(rstd[:, g0:g0 + len(tiles)], ss[:, g0:g0 + len(tiles)], len(tiles), 1.0 / D, [(tag + "ss", i) for i in tiles],
                          [tag + "rstd%d" % g], rtmp[:, g0:g0 + len(tiles)])

            def st_b(g):
                base = (g % 2) * 4
                for ii, i in enumerate(tiles_of(g)):
                    ap, rd = src_tile(i)
                    xb = xsb[i % 2]
                    TS(xb, ap, rstd[:, i:i + 1], None, ALU.mult, None, rd + [tag + "rstd%d" % g], [(tag + "xs", i % 2)])
                    lst = []
                    for k in range(8):
                        lst.append((PSB[base + k // 2][:, (k % 2) * 512 + ii * 128:(k % 2) * 512 + (ii + 1) * 128],
                                    xb[:, k * 128:(k + 1) * 128], ident))
                    TRG(lst, [(tag + "xs", i % 2), "ident"], [psr(base + kk) for kk in range(4)])

            def st_c(g):
                base = (g % 2) * 4
                nt_ = len(tiles_of(g))
                for k in range(8):
                    o_ = dstT[:, k, g * 512:g * 512 + nt_ * 128]
                    i_ = PSB[base + k // 2][:, (k % 2) * 512:(k % 2) * 512 + nt_ * 128]
                    if k % 2 == 0:
                        ACT(o_, i_, AF.Identity, [psr(base + k // 2)] + sc_res, [(tag + "T", k, g)], scale=sc_ap[:, k:k + 1], bias=bi_ap[:, k:k + 1])
                    else:
                        TS(o_, i_, sc_ap[:, k:k + 1], bi_ap[:, k:k + 1], ALU.mult, ALU.add, [psr(base + k // 2)] + sc_res, [(tag + "T", k, g)])

            sts = [st_a, st_b, st_c]
            for step in range(ngrp + 2):
                for kk in (2, 1, 0):
                    if 0 <= step - kk < ngrp:
                        sts[kk](step - kk)

        hcT = A.alloc(8 * CTX, BF16).rearrange("p (k t) -> p k t", k=8)
        P1M = A.mark()
        xt = [A.alloc(1024) for _ in range(8)]
        junk = A.alloc(1024)
        xsb = [A.alloc(1024, BF16) for _ in range(2)]
        ss = A.alloc(16); rstd = A.alloc(16); rtmp = A.alloc(16)
        ssc = A.alloc(2); rstdc = A.alloc(2); rtmpc = A.alloc(2)
        x_v = T["x"].rearrange("(i p) d -> p i d", p=128)
        c_v = T["ctx"].rearrange("(i p) d -> p i d", p=128)

        def ctx_tile(i):
            return xt[i % 8], [("xt", i % 8)]
        for i in range(2):
            S.dma(xt[i % 8], c_v[:, i, :], writes=[("xt", i % 8)])
        norm_to_T(ctx_tile, 2, cs1, cb1, hcT, "c", ["cs1", "cb1"], junk, xsb, ssc, rstdc, rtmpc)

        def x_tile(i):
            return xt[i % 8], [("xt", i % 8)]
        loaded = set()

        def x_tile_load(i):
            if i not in loaded:
                loaded.add(i)
                S.dma(xt[i % 8], x_v[:, i, :], writes=[("xt", i % 8)])
            return x_tile(i)
        norm_to_T(x_tile_load, 16, s1, b1, hT, "h", ["s1", "b1"], junk, xsb, ss, rstd, rtmp)
        HT_RES = [("hT", k, g) for k in range(8) for g in range(4)]
        DUMP("hT", hT, [128, 8, L], HT_RES)
        DUMP("hcT", hcT, [128, 8, CTX], [("cT", k, 0) for k in range(8)])
        if stop_after <= 1:
            S.emit(st)
            return nc, dbg_specs, S
        S.barrier()
        A.reset(P1M)

        win_v = T["w_in"].rearrange("(k p) n -> p k n", p=128)
        wch = [A.alloc(8 * 512, BF16).rearrange("p (k n) -> p k n", k=8) for _ in range(2)]

        def load_wchunk(slot, col0):
            for hk in range(2):
                S.dma(wch[slot][:, hk * 4:(hk + 1) * 4, :], win_v[:, hk * 4:(hk + 1) * 4, col0:col0 + 512],
                      writes=[("wch", slot, k) for k in range(hk * 4, hk * 4 + 4)], eng="pool")
            return [("wch", slot, k) for k in range(8)]

        ytab = yT.rearrange("p k t -> p (k t)")[:, 0:4 * L].bitcast(F32)
        MTt = ytab[:, 0:512].rearrange("p (h i) -> p h i", h=4)
        ropeC = ytab[:, 512:1536].rearrange("p (n f) -> p n f", n=16)
        ropeS = ytab[:, 1536:2560].rearrange("p (n f) -> p n f", n=16)
        zf = ytab[:, 2560:3072]; zb = ytab[:, 3072:3584]
        xi = A.alloc(8)
        gtab = A.alloc(1024).rearrange("p (d n) -> p d n", d=2)
        gnwb = A.alloc(512)
        Sf = A.alloc(512); Sb = A.alloc(512); Stmp = A.alloc(512)
        P2M = A.mark()
        ctxw = A.alloc(2048).rearrange("p (t d n) -> p t d n", t=2, d=2)
        ckf = [A.alloc(512, BF16) for _ in range(2)]
        ckb = [A.alloc(512, BF16) for _ in range(2)]
        cv = [A.alloc(512, BF16) for _ in range(2)]
        for nm, ap in (("MT", MTt), ("ropeC", ropeC), ("ropeS", ropeS), ("zf", zf), ("zb", zb), ("xi", xi), ("gtab", gtab), ("ctxw", ctxw)):
            S.dma(ap, T[nm], writes=[nm])
        S.dma(gnwb, bass.AP(T["gnw"].tensor, 0, [[0, 128], [1, 512]]), writes=["gnwb"])
        rk_k = load_wchunk(0, 2048)
        rk_v = load_wchunk(1, 2560)

        for ci in range(2):
            lst = [(PS[0][:, :], hcT[:, k, ci * 128:(ci + 1) * 128], wch[0][:, k, :], k == 0, k == 7) for k in range(8)]
            MMG(lst, rk_k + [("cT", k, 0) for k in range(8)], [psr(0)])
            lst = [(PS[1][:, :], hcT[:, k, ci * 128:(ci + 1) * 128], wch[1][:, k, :], k == 0, k == 7) for k in range(8)]
            MMG(lst, rk_v + [("cT", k, 0) for k in range(8)], [psr(1)])
            TT(ckf[ci], PS[0][:, :], ctxw[:, ci, 0, :], ALU.mult, [psr(0), "ctxw"], [("ckf", ci)])
            TT(ckb[ci], PS[0][:, :], ctxw[:, ci, 1, :], ALU.mult, [psr(0), "ctxw"], [("ckb", ci)])
            ACT(cv[ci], PS[1][:, :], AF.Copy, [psr(1)], [("cv", ci)])
        for dr, (kk, Sx, nm) in enumerate(((ckf, Sf, "Sf"), (ckb, Sb, "Sb"))):
            lst = []
            for h in range(4):
                for ci in range(2):
                    lst.append((PS[2 + dr][:, h * 128:(h + 1) * 128], kk[ci][:, h * 128:(h + 1) * 128], cv[ci][:, h * 128:(h + 1) * 128], ci == 0, ci == 1))
            MMG(lst, [("ckf", 0), ("ckf", 1), ("ckb", 0), ("ckb", 1), ("cv", 0), ("cv", 1)], [psr(2 + dr)])
            CP(Sx, PS[2 + dr][:, :], [psr(2 + dr)], [nm])
        DUMP("S0f", Sf, [128, 512], ["Sf"])
        DUMP("S0b", Sb, [128, 512], ["Sb"])
        if stop_after <= 1.5:
            S.emit(st)
            return nc, dbg_specs, S
        S.barrier()
        A.reset(P2M)
        kT = A.alloc(4 * L, BF16).rearrange("p (h t) -> p h t", h=4)
        k_tok = A.alloc(16 * 512, BF16).rearrange("p (n c) -> p n c", n=16)
        v_tok = A.alloc(16 * 512, BF16).rearrange("p (n c) -> p n c", n=16)
        SB = A.alloc(16 * 512, BF16).rearrange("p (n c) -> p n c", n=16)
        Sf_bf = A.alloc(512, BF16)
        qk_tok = [A.alloc(512, BF16) for _ in range(2)]
        rt1 = A.alloc(256); rt2 = A.alloc(256)
        kz = [A.alloc(512, BF16) for _ in range(2)]
        qTt = [A.alloc(512, BF16).rearrange("p (h t) -> p h t", h=4) for _ in range(2)]
        masked = [A.alloc(512, BF16).rearrange("p (h i) -> p h i", h=4) for _ in range(2)]
        sg_t = [A.alloc(512) for _ in range(4)]
        qfT = [A.alloc(512, BF16).rearrange("p (h t) -> p h t", h=4) for _ in range(2)]
        qbT = [A.alloc(512, BF16).rearrange("p (h t) -> p h t", h=4) for _ in range(2)]
        xiT = A.alloc(1024).rearrange("p (d h t) -> p d h t", d=2, h=4)
        on_t = A.alloc(512)
        yr_t = [A.alloc(512, BF16) for _ in range(2)]
        bst = A.alloc(24); mv = A.alloc(8); grs = A.alloc(4); grt = A.alloc(4)
        rk_k = [("wch", 0, k) for k in range(8)]
        rk_v = [("wch", 1, k) for k in range(8)]

        def rope(dst_bf, src_ps, n, ps_res, tagw):
            ACT(dst_bf, src_ps, AF.Copy, [ps_res], [tagw])
            srcv = src_ps.rearrange("p (h c) -> p h c", h=4)[:, :, 0:64]
            cb = bview(ropeC, n * 64, [[0, 4], [1, 64]])
            sb_ = bview(ropeS, n * 64, [[0, 4], [1, 64]])
            t1 = rt1.rearrange("p (h c) -> p h c", h=4)
            t2 = rt2.rearrange("p (h c) -> p h c", h=4)
            TT(t1, srcv, cb, ALU.mult, [ps_res, "ropeC"], ["rt1"])
            TT(t2, srcv, sb_, ALU.mult, [ps_res, "ropeS"], ["rt2"])
            d4 = dst_bf.rearrange("p (h x a f) -> p h x a f", h=4, x=4, a=2)
            t14 = rt1.rearrange("p (h x a f) -> p h x a f", h=4, x=2, a=2)
            t24 = rt2.rearrange("p (h x a f) -> p h x a f", h=4, x=2, a=2)
            TT(d4[:, :, 0:2, 0, :], t14[:, :, :, 0, :], t24[:, :, :, 1, :], ALU.subtract, ["rt1", "rt2"], [tagw])
            TT(d4[:, :, 0:2, 1, :], t14[:, :, :, 1, :], t24[:, :, :, 0, :], ALU.add, ["rt1", "rt2"], [tagw])

        CP(SB[:, 15, :], Sb, ["Sb"], [("SB", 15)])
        hres = lambda n: [("hT", k, n // 4) for k in range(8)]
        def sa1(n):
            pk, pv = 0 + (n % 2) * 4, 1 + (n % 2) * 4
            lst = [(PS[pk][:, :], hT[:, k, n * 128:(n + 1) * 128], wch[0][:, k, :], k == 0, k == 7) for k in range(8)]
            MMG(lst, rk_k + hres(n), [psr(pk)])
            lst = [(PS[pv][:, :], hT[:, k, n * 128:(n + 1) * 128], wch[1][:, k, :], k == 0, k == 7) for k in range(8)]
            MMG(lst, rk_v + hres(n), [psr(pv)])
            ACT(v_tok[:, n, :], PS[pv][:, :], AF.Copy, [psr(pv)], [("v_tok", n)])
            rope(k_tok[:, n, :], PS[pk][:, :], n, psr(pk), ("k_tok", n))

        def sa2(n):
            pt = 2 + (n % 2) * 4
            lst = [(PSB[pt][:, h * 128:(h + 1) * 128], k_tok[:, n, h * 128:(h + 1) * 128], ident) for h in range(4)]
            TRG(lst, [("k_tok", n), "ident"], [psr(pt)])
            CP(kT[:, :, n * 128:(n + 1) * 128], PSB[pt][:, 0:512].rearrange("p (h t) -> p h t", h=4), [psr(pt)], [("kT", n)])
            if n > 0:
                TT(kz[n % 2], k_tok[:, n, :], zb, ALU.mult, [("k_tok", n), "zb"], [("kz", n % 2)], eng="pool")

        def sa3(n):
            pkv = 3 + (n % 2) * 4
            if n > 0:
                kzb = kz[n % 2]
                lst = [(PS[pkv][:, h * 128:(h + 1) * 128], kzb[:, h * 128:(h + 1) * 128], v_tok[:, n, h * 128:(h + 1) * 128], True, True) for h in range(4)]
                MMG(lst, [("kz", n % 2), ("v_tok", n)], [psr(pkv)])
                TT(Stmp, Sb, gtab[:, 1, :], ALU.mult, ["Sb", "gtab"], ["Stmp"], eng="pool")
                TT(Sb, PS[pkv][:, :], Stmp, ALU.add, [psr(pkv), "Stmp"], ["Sb"])
                ACT(SB[:, n - 1, :], Sb, AF.Copy, ["Sb"], [("SB", n - 1)])

        stages_a = [sa1, sa2, sa3]
        for step in range(16 + 2):
            for kk in (2, 1, 0):
                if 0 <= step - kk < 16:
                    stages_a[kk](15 - (step - kk))
        DUMP("kT", kT, [128, 4, L], [("kT", n) for n in range(16)])
        DUMP("SB", SB, [128, 16, 512], [("SB", n) for n in range(16)])
        if stop_after <= 1.7:
            S.emit(st)
            return nc, dbg_specs, S

        rk_q = load_wchunk(0, 1536)
        rk_g = load_wchunk(1, 3072)
        S.dma(xiT, T["xiT"], writes=["xiT"])
        ACT(Sf_bf, Sf, AF.Copy, ["Sf"], ["Sf_bf"])

        def sb1(n):
            b = n % 2
            lst = [(PS[0][:, :], hT[:, k, n * 128:(n + 1) * 128], wch[0][:, k, :], k == 0, k == 7) for k in range(8)]
            MMG(lst, rk_q + hres(n), [psr(0)])
            rope(qk_tok[b], PS[0][:, :], n, psr(0), ("q_tok", b))
            lst = [(PS[2][:, :], hT[:, k, n * 128:(n + 1) * 128], wch[1][:, k, :], k == 0, k == 7) for k in range(8)]
            MMG(lst, rk_g + hres(n), [psr(2)])
            ACT(sg_t[n % 4], PS[2][:, :], AF.Silu, [psr(2)], [("sg", n % 4)])

        def sb2(n):
            b = n % 2
            lst = [(PSB[1][:, h * 128:(h + 1) * 128], qk_tok[b][:, h * 128:(h + 1) * 128], ident) for h in range(4)]
            TRG(lst, [("q_tok", b), "ident"], [psr(1)])
            CP(qTt[b], PSB[1][:, 0:512].rearrange("p (h t) -> p h t", h=4), [psr(1)], [("qT", b)])
            if n < 15:
                TT(kz[b], k_tok[:, n, :], zf, ALU.mult, [("k_tok", n), "zf"], [("kz", b)], eng="pool")
            TT(qfT[b], qTt[b], xiT[:, 0, :, :], ALU.mult, [("qT", b), "xiT"], [("qfT", b)], eng="pool")
            TT(qbT[b], qTt[b], xiT[:, 1, :, :], ALU.mult, [("qT", b), "xiT"], [("qbT", b)], eng="pool")

        def sb2b(n):
            b = n % 2
            lst = [(PS[3][:, h * 128:(h + 1) * 128], kT[:, h, n * 128:(n + 1) * 128], qTt[b][:, h, :], True, True) for h in range(4)]
            MMG(lst, [("kT", n), ("qT", b)], [psr(3)])
            TT(masked[b], PS[3][:, :].rearrange("p (h i) -> p h i", h=4), MTt, ALU.mult, [psr(3), "MT"], [("masked", b)])

        def sb3(n):
            b = n % 2
            po = 4 + b
            if n < 15:
                lst = [(PS[7][:, h * 128:(h + 1) * 128], kz[b][:, h * 128:(h + 1) * 128], v_tok[:, n, h * 128:(h + 1) * 128], True, True) for h in range(4)]
                MMG(lst, [("kz", b), ("v_tok", n)], [psr(7)])
            lst = []
            for h in range(4):
                hs = slice(h * 128, (h + 1) * 128)
                lst.append((PS[po][:, hs], masked[b][:, h, :], v_tok[:, n, hs], True, False))
                lst.append((PS[po][:, hs], qfT[b][:, h, :], Sf_bf[:, hs], False, False))
                lst.append((PS[po][:, hs], qbT[b][:, h, :], SB[:, n, hs], False, True))
            MMG(lst, [("masked", b), ("v_tok", n), ("qfT", b), ("qbT", b), "Sf_bf", ("SB", n)], [psr(po)])
            if n < 15:
                TT(Stmp, Sf, gtab[:, 0, :], ALU.mult, ["Sf", "gtab"], ["Stmp"], eng="pool")
                TT(Sf, PS[7][:, :], Stmp, ALU.add, [psr(7), "Stmp"], ["Sf"])
                ACT(Sf_bf, Sf, AF.Copy, ["Sf"], ["Sf_bf"])

        def sb4(n):
            b = n % 2
            po = 4 + b
            for h in range(4):
                S.dve(lambda e, h=h, po=po: e.bn_stats(out=bst[:, h * 6:(h + 1) * 6], in_=PS[po][:, h * 128:(h + 1) * 128]), [psr(po)], [("bst", h)])
                S.dve(lambda e, h=h: e.bn_aggr(out=mv[:, h * 2:(h + 1) * 2], in_=bst[:, h * 6:(h + 1) * 6]), [("bst", h)], [("mv", h)])
            mvv = mv.rearrange("p (h c) -> p h c", c=2)
            rsqrt_ops(grs, mvv[:, :, 1], 4, 1.0, [("mv", h) for h in range(4)], ["grs"], grt)
            for h in range(4):
                TS(on_t[:, h * 128:(h + 1) * 128], PS[po][:, h * 128:(h + 1) * 128], mv[:, 2 * h:2 * h + 1], grs[:, h:h + 1],
                   ALU.subtract, ALU.mult, [psr(po), ("mv", h), "grs"], ["on"])
            TT(on_t, on_t, gnwb, ALU.mult, ["on", "gnwb"], ["on"], eng="pool")
            TT(yr_t[b], on_t, sg_t[n % 4], ALU.mult, ["on", ("sg", n % 4)], [("yr", b)])

        def sb5(n):
            b = n % 2
            lst = [(PSB[6][:, h * 128:(h + 1) * 128], yr_t[b][:, h * 128:(h + 1) * 128], ident) for h in range(4)]
            TRG(lst, [("yr", b), "ident"], [psr(6)])
            ACT(yT[:, 4:8, n * 128:(n + 1) * 128], PSB[6][:, 0:512].rearrange("p (h t) -> p h t", h=4), AF.Copy, [psr(6)], [("yT", "r", n)])

        stages_b = [sb1, sb2, sb2b, sb3, sb4, sb5]
        for step in range(16 + len(stages_b) - 1):
            for k in range(len(stages_b) - 1, -1, -1):
                if 0 <= step - k < 16:
                    stages_b[k](step - k)
        DUMP("yTr", yT[:, 4:8, :], [128, 4, L], [("yT", "r", n) for n in range(16)])
        S.barrier()
        A.reset(PERS)
        if stop_after <= 2:
            S.emit(st)
            return nc, dbg_specs, S

        pq_p = A.alloc(16 * 512, BF16).rearrange("p (n c) -> p n c", n=16)
        pq_q = A.alloc(16 * 512, BF16).rearrange("p (n c) -> p n c", n=16)
        P3A = A.mark()
        zT = A.alloc(L)
        fa = [A.alloc(L) for _ in range(2)]
        ftmp = A.alloc(L)
        fw1 = A.alloc(64); fw2 = A.alloc(64); fw3 = A.alloc(64)
        fwo = A.alloc(1024)
        fb = A.alloc(3); ffr = A.alloc(1); ffb = A.alloc(3)
        skip_t = A.alloc(512)
        winf = [A.alloc(512) for _ in range(2)]
        winb = [A.alloc(512) for _ in range(2)]
        hf_t = A.alloc(512); hb_t = A.alloc(512)
        S.dma(zT[0:33, :], T["zT"], writes=["zT"])
        S.dma(fw1[0:33, :], T["f_w1"], writes=["fw1"])
        S.dma(fw2[0:64, :], T["f_w2"], writes=["fw2"])
        S.dma(fw3[0:64, :], T["f_w3"], writes=["fw3"])
        S.dma(fwo[0:64, :], T["f_wout"], writes=["fwo"])
        S.dma(fb[0:64, :], T["f_b"], writes=["fb"])
        S.dma(ffr[0:64, :], T["f_freq"], writes=["ffr"])
        S.dma(skip_t[0:1, :], T["hy_skip"], writes=["skip"])
        TS(ffb[0:64, :], fb[0:64, :], ffr[0:64, 0:1], None, ALU.mult, None, ["fb", "ffr"], ["ffb"])
        adaw2 = [A.alloc(8 * 1024, BF16).rearrange("p (k n) -> p k n", k=8) for _ in range(2)]
        adabrow2 = [A.alloc(1024) for _ in range(2)]
        lhsb2 = A.alloc(8 * 128, BF16).rearrange("p (k m) -> p k m", k=8)
        for k in range(8):
            ACT(lhsb2[:, k, :], ones_f, AF.Identity, [], [("lhsb", k)], scale=sc_f[:, k, 0:1])
        ada_dma(2, adaw2[0], 0)
        ada_dma(3, adaw2[1], 1)
        layers = [(fw1, 33, zT, "zT", "fw1"), (fw2, 64, fa[0], ("fa", 0), "fw2"), (fw3, 64, fa[1], ("fa", 1), "fw3")]
        for li, (w, kdim, src, sres, wres) in enumerate(layers):
            dst = fa[li % 2]
            dres = ("fa", li % 2)
            for tcn in range(4):
                bank = tcn % 2
                MMG([(PS[bank][0:64, :], w[0:kdim, :], src[0:kdim, tcn * 512:(tcn + 1) * 512], True, True)], [sres, wres], [psr(bank)])
                ACT(dst[0:64, tcn * 512:(tcn + 1) * 512], PS[bank][0:64, :], AF.Identity, [psr(bank), "ffr", "ffb"], [(dres, tcn)],
                    scale=ffr[0:64, 0:1], bias=ffb[0:64, li:li + 1])
            allr = [(dres, tcn) for tcn in range(4)]
            TS(ftmp[0:64, :], dst[0:64, :], 1.0 / TWO_PI, MAGIC, ALU.mult, ALU.add, allr, ["ftmp"])
            TS(ftmp[0:64, :], ftmp[0:64, :], MAGIC, -TWO_PI, ALU.subtract, ALU.mult, ["ftmp"], ["ftmp"])
            TT(dst[0:64, :], dst[0:64, :], ftmp[0:64, :], ALU.add, allr + ["ftmp"], [dres])
            ACT(dst[0:64, :], dst[0:64, :], AF.Sin, [dres], [dres])
        h3 = fa[0]
        ada_mm(2, adaw2[0], 0, lhsb2, adabrow2[0])
        ada_mm(3, adaw2[1], 1, lhsb2, adabrow2[0])
        ada_dma(4, adaw2[0], 0)
        ada_dma(5, adaw2[1], 1)
        DUMP("h3", h3[0:64, :], [64, L], [("fa", 0)])
        for t16 in range(16):
            par, jb = t16 // 8, t16 % 8
            b = t16 % 2
            lhs = bview(h3[0:64, :], par + 256 * jb, [[2, 128]])
            S.dma(winf[b], T["winF"][t16], writes=[("winf", b)])
            S.dma(winb[b], T["winB"][t16], writes=[("winb", b)])
            for half in range(2):
                MMG([(PS[2 + half + 2 * b][:, :], lhs, fwo[0:64, half * 512:(half + 1) * 512], True, True)], [("fa", 0), "fwo"], [psr(2 + half + 2 * b)])
            TT(hf_t, PS[2 + 2 * b][:, :], winf[b], ALU.mult, [psr(2 + 2 * b), ("winf", b)], ["hf"])
            TT(hb_t, PS[3 + 2 * b][:, :], winb[b], ALU.mult, [psr(3 + 2 * b), ("winb", b)], ["hb"])
            if t16 == 0:
                TT(hf_t[0:1, :], hf_t[0:1, :], skip_t[0:1, :], ALU.add, ["hf", "skip"], ["hf"])
            TT(pq_p[:, t16, :], hf_t, hb_t, ALU.add, ["hf", "hb"], [("pq_p", t16)], eng="pool")
            TT(pq_q[:, t16, :], hb_t, hf_t, ALU.subtract, ["hf", "hb"], [("pq_q", t16)], eng="pool")
        ada_mm(4, adaw2[0], 0, lhsb2, adabrow2[1])
        ada_mm(5, adaw2[1], 1, lhsb2, adabrow2[1])
        STT(s2, modc[:, 24:32], 1.0, nw2, ALU.add, ALU.mult, [("modc", 3)], ["s2"])
        CP(b2, modc[:, 16:24], [("modc", 2)], ["b2"])
        DUMP("g1b", g1b, [128, 1024], [("g1b", 0), ("g1b", 1)])
        DUMP("pq_p", pq_p, [128, 16, 512], [("pq_p", t) for t in range(16)])
        DUMP("pq_q", pq_q, [128, 16, 512], [("pq_q", t) for t in range(16)])
        S.barrier()
        A.reset(P3A)
        if stop_after <= 3:
            S.emit(st)
            return nc, dbg_specs, S

        vx_tok = A.alloc(16 * 512, BF16).rearrange("p (n c) -> p n c", n=16)
        P3B = A.mark()
        wch = [A.alloc(8 * 512, BF16).rearrange("p (k n) -> p k n", k=8) for _ in range(2)]
        convw = A.alloc(36).rearrange("p (c t) -> p c t", t=3)
        convb = A.alloc(12)
        S.dma(convw, T["convw"], writes=["convw"])
        S.dma(convb, T["convb"], writes=["convb"])
        pT = [A.alloc(L + 2) for _ in range(2)]
        u_x1 = A.alloc(L)
        u_v = A.alloc(L)
        vxT = A.alloc(4 * L, BF16).rearrange("p (c t) -> p c t", c=4)
        for i in range(2):
            S.dve(lambda e, p=pT[i]: e.memset(p[:, 0:1], 0.0), writes=[("pT", i)])
            S.dve(lambda e, p=pT[i]: e.memset(p[:, L + 1:L + 2], 0.0), writes=[("pT", i)])

        def conv_chunk(slot, cl, cg, dst, dres, pidx, tmp=None, tres=None):
            rk = [("wch", slot, k) for k in range(8)]
            p = pT[pidx]
            for tcn in range(4):
                bank = (tcn % 2) + 2 * pidx
                lst = [(PS[bank][:, :], wch[slot][:, k, cl * 128:(cl + 1) * 128], hT[:, k, tcn * 512:(tcn + 1) * 512], k == 0, k == 7) for k in range(8)]
                MMG(lst, rk + [("hT", k, tcn) for k in range(8)], [psr(bank)])
                ACT(p[:, 1 + tcn * 512:1 + (tcn + 1) * 512], PS[bank][:, :], AF.Copy, [psr(bank)], [("pT", pidx)])
            if tmp is None:
                tmp, tres = dst, dres
            ACT(tmp, p[:, 1:L + 1], AF.Identity, [("pT", pidx), "convw", "convb"], [tres], scale=convw[:, cg, 1:2], bias=convb[:, cg:cg + 1])
            STT(tmp, p[:, 0:L], convw[:, cg, 0:1], tmp, ALU.mult, ALU.add, [("pT", pidx), "convw", tres], [tres])
            STT(dst, p[:, 2:L + 2], convw[:, cg, 2:3], tmp, ALU.mult, ALU.add, [("pT", pidx), "convw", tres], [dres])

        load_wchunk(0, 512)
        load_wchunk(1, 1024)
        for cl in range(4):
            conv_chunk(0, cl, 4 + cl, u_x1, "u_x1", 0)
            conv_chunk(1, cl, 8 + cl, u_v, "u_v", 1)
            TT(vxT[:, cl, :], u_x1, u_v, ALU.mult, ["u_x1", "u_v"], [("vxT", cl)], eng="pool")
        DUMP("vxT", vxT, [128, 4, L], [("vxT", c) for c in range(4)])
        for t16 in range(16):
            par, jb = t16 // 8, t16 % 8
            bank = 4 + (t16 % 4)
            lst = [(PSB[bank][:, c * 128:(c + 1) * 128], bview(vxT, c * L + par + 256 * jb, [[2, 128]]), ident) for c in range(4)]
            TRG(lst, [("vxT", c) for c in range(4)] + ["ident"], [psr(bank)])
            if t16 % 2 == 0:
                ACT(vx_tok[:, t16, :], PSB[bank][:, 0:512], AF.Copy, [psr(bank)], [("vx_tok", t16)])
            else:
                CP(vx_tok[:, t16, :], PSB[bank][:, 0:512], [psr(bank)], [("vx_tok", t16)])
        S.barrier()
        A.reset(P3B)
        if stop_after <= 4:
            S.emit(st)
            return nc, dbg_specs, S

        PQ = A.alloc(8 * 4 * 512, BF16).rearrange("p (j s c) -> p j s c", j=8, s=4)
        PQ_END = A.mark()
        Fb = [(A.alloc(16 * 128, BF16).rearrange("p (t f) -> p t f", t=16), A.alloc(16 * 128, BF16).rearrange("p (t f) -> p t f", t=16)) for _ in range(2)]
        XsB = [[A.alloc(512, BF16) for _ in range(4)] for _ in range(2)]
        HsB = [[A.alloc(512, BF16) for _ in range(4)] for _ in range(2)]
        tmpB = [[A.alloc(512, BF16) for _ in range(4)] for _ in range(2)]
        _oc = [A.alloc(512, BF16) for _ in range(4)]
        oc_t = [_oc, _oc]
        for j in range(8):
            fb_ = j % 2
            Xs, Hs, tmp4, occ = XsB[fb_], HsB[fb_], tmpB[fb_], oc_t[fb_]
            X = lambda k: ("X", fb_, k)
            Hn = lambda k: ("H", fb_, k)
            Tn = lambda k: ("t", fb_, k)
            On = lambda k: ("oc", k)
            Fcj, Fsj = Fb[fb_]
            S.dma(Fcj, T["Fc"][j], writes=[("Fc", fb_)])
            S.dma(Fsj, T["Fs"][j], writes=[("Fs", fb_)])
            for bi, (Ft, fres, t0) in enumerate(((Fcj, ("Fc", fb_), 0), (Fcj, ("Fc", fb_), 8), (Fsj, ("Fs", fb_), 0), (Fsj, ("Fs", fb_), 8))):
                lst = [(PS[bi][:, :], Ft[:, t0 + i, :], vx_tok[:, t0 + i, :], i == 0, i == 7) for i in range(8)]
                MMG(lst, [fres] + [("vx_tok", t0 + i) for i in range(8)], [psr(bi)])
            for bi, (Ft, fres, t0, src, sn) in enumerate(((Fcj, ("Fc", fb_), 0, pq_p, "pq_p"), (Fcj, ("Fc", fb_), 8, pq_p, "pq_p"),
                                                            (Fsj, ("Fs", fb_), 0, pq_q, "pq_q"), (Fsj, ("Fs", fb_), 8, pq_q, "pq_q"))):
                lst = [(PS[4 + bi][:, :], Ft[:, t0 + i, :], src[:, t0 + i, :], i == 0, i == 7) for i in range(8)]
                MMG(lst, [fres] + [(sn, t0 + i) for i in range(8)], [psr(4 + bi)])
            ACT(occ[0], PS[1][:, :], AF.Copy, [psr(1)], [On(0)])
            ACT(occ[1], PS[3][:, :], AF.Copy, [psr(3)], [On(1)])
            TT(Xs[0], PS[0][:, :], occ[0], ALU.add, [psr(0), On(0)], [X(0)])
            TT(Xs[1], PS[0][:, :], occ[0], ALU.subtract, [psr(0), On(0)], [X(1)])
            TT(Xs[2], PS[2][:, :], occ[1], ALU.add, [psr(2), On(1)], [X(2)])
            STT(Xs[3], PS[2][:, :], -1.0, occ[1], ALU.mult, ALU.add, [psr(2), On(1)], [X(3)])
            ACT(occ[2], PS[5][:, :], AF.Copy, [psr(5)], [On(2)])
            ACT(occ[3], PS[7][:, :], AF.Copy, [psr(7)], [On(3)])
            TT(Hs[0], PS[4][:, :], occ[2], ALU.add, [psr(4), On(2)], [Hn(0)])
            TT(Hs[1], PS[4][:, :], occ[2], ALU.subtract, [psr(4), On(2)], [Hn(1)])
            TT(Hs[2], PS[6][:, :], occ[3], ALU.add, [psr(6), On(3)], [Hn(2)])
            STT(Hs[3], PS[6][:, :], -1.0, occ[3], ALU.mult, ALU.add, [psr(6), On(3)], [Hn(3)])
            for fm in range(2):
                Cx, Sx, Hr, Hi = Xs[fm], Xs[2 + fm], Hs[fm], Hs[2 + fm]
                rC, rS, rHr, rHi = X(fm), X(2 + fm), Hn(fm), Hn(2 + fm)
                e1 = "pool" if fm == 0 else "dve"
                TT(tmp4[0], Cx, Hr, ALU.mult, [rC, rHr], [Tn(0)], eng=e1)
                TT(tmp4[1], Sx, Hi, ALU.mult, [rS, rHi], [Tn(1)], eng=e1)
                TT(tmp4[2], Cx, Hi, ALU.mult, [rC, rHi], [Tn(2)], eng="pool")
                TT(tmp4[3], Sx, Hr, ALU.mult, [rS, rHr], [Tn(3)], eng="pool")
                TT(Cx, tmp4[0], tmp4[1], ALU.add, [Tn(0), Tn(1)], [rC], eng=e1)
                TT(Sx, tmp4[2], tmp4[3], ALU.subtract, [Tn(2), Tn(3)], [rS], eng="pool")
            TT(PQ[:, j, 0, :], Xs[0], Xs[1], ALU.add, [X(0), X(1)], [("PQ", j)])
            TT(PQ[:, j, 1, :], Xs[2], Xs[3], ALU.subtract, [X(2), X(3)], [("PQ", j)])
            TT(PQ[:, j, 2, :], Xs[0], Xs[1], ALU.subtract, [X(0), X(1)], [("PQ", j)], eng="pool")
            TT(PQ[:, j, 3, :], Xs[2], Xs[3], ALU.add, [X(2), X(3)], [("PQ", j)], eng="pool")
        DUMP("PQ", PQ, [128, 8, 4, 512], [("PQ", j) for j in range(8)])
        S.barrier()
        if stop_after <= 5:
            S.emit(st)
            return nc, dbg_specs, S

        A.reset(PERS)
        x0T = A.alloc(4 * L, BF16).rearrange("p (c t) -> p c t", c=4)
        wch = [A.alloc(8 * 512, BF16).rearrange("p (k n) -> p k n", k=8)]
        convw = A.alloc(36).rearrange("p (c t) -> p c t", t=3)
        convb = A.alloc(12)
        pT = [A.alloc(L + 2) for _ in range(1)]
        u0 = A.alloc(L)
        Gb = []
        assert A.top <= PERS + 3 * 16 * 256, "P3d scratch must stay below PQ"
        S.dma(convw, T["convw"], writes=["convw"])
        S.dma(convb, T["convb"], writes=["convb"])
        for i in range(1):
            S.dve(lambda e, p=pT[i]: e.memset(p[:, 0:1], 0.0), writes=[("pT", i)])
            S.dve(lambda e, p=pT[i]: e.memset(p[:, L + 1:L + 2], 0.0), writes=[("pT", i)])
        load_wchunk(0, 0)
        for cl in range(4):
            conv_chunk(0, cl, cl, x0T[:, cl, :], ("x0T", cl), 0, tmp=u0, tres="u0")
        DUMP("x0T", x0T, [128, 4, L], [("x0T", c) for c in range(4)])
        A.reset(PQ_END)
        Gb.append((A.alloc(8 * 512, BF16).rearrange("p (j t) -> p j t", j=8), A.alloc(8 * 512, BF16).rearrange("p (j t) -> p j t", j=8)))
        Gb.append((A.alloc(8 * 512, BF16).rearrange("p (j t) -> p j t", j=8), A.alloc(8 * 512, BF16).rearrange("p (j t) -> p j t", j=8)))
        sq_t = [A.alloc(512, BF16) for _ in range(2)]
        rstdb = A.alloc(L)
        rtmpb = A.alloc(512)
        hynw = A.alloc(4)
        S.dma(hynw, T["hynw"], writes=["hynw"])
        epsb = A.alloc(1)
        S.dve(lambda e: e.memset(epsb, EPS), writes=["epsb"])
        for oc in range(4):
            par, half = oc // 2, oc % 2
            Gcj, Gsj = Gb[oc % 2]
            S.dma(Gcj, T["Gc"][oc], writes=[("Gc", oc % 2)])
            S.dma(Gsj, T["Gs"][oc], writes=[("Gs", oc % 2)])
            so = 0 if par == 0 else 2
            for c in range(4):
                bank = (oc * 4 + c) % 4
                lst = []
                for j in range(8):
                    lst.append((PS[bank][:, :], PQ[:, j, so, c * 128:(c + 1) * 128], Gcj[:, j, :], j == 0, False))
                    lst.append((PS[bank][:, :], PQ[:, j, so + 1, c * 128:(c + 1) * 128], Gsj[:, j, :], False, j == 7))
                MMG(lst, [("PQ", j) for j in range(8)] + [("Gc", oc % 2), ("Gs", oc % 2)], [psr(bank)])
                x0v = bview(x0T, c * L + par + 1024 * half, [[2, 512]])
                zv = bview(yT, c * L + par + 1024 * half, [[2, 512]])
                TT(zv, PS[bank][:, :], x0v, ALU.mult, [psr(bank), ("x0T", c)], [("z", c, oc)])
        DUMP("zT", yT[:, 0:4, :], [128, 4, L], [("z", c, oc) for c in range(4) for oc in range(4)])
        for tcn in range(4):
            zres = [("z", c, oc) for c in range(4) for oc in range(4)]
            lst = []
            for c in range(4):
                sq = sq_t[c % 2]
                TT(sq, yT[:, c, tcn * 512:(tcn + 1) * 512], yT[:, c, tcn * 512:(tcn + 1) * 512], ALU.mult, zres, [("sq", c % 2)], eng="pool")
                MMG([(PS[4 + tcn % 2][:, :], ones_bf, sq, c == 0, c == 3)], [("sq", c % 2), "ones_bf"], [psr(4 + tcn % 2)])
            ACT(rtmpb, PS[4 + tcn % 2][:, :], AF.Ln, [psr(4 + tcn % 2), "epsb"], ["rtmpb"], scale=1.0 / 512, bias=epsb[:, 0:1])
            ACT(rstdb[:, tcn * 512:(tcn + 1) * 512], rtmpb, AF.Exp, ["rtmpb"], [("rstdb", tcn)], scale=-0.5)
            for c in range(4):
                STT(yT[:, c, tcn * 512:(tcn + 1) * 512], yT[:, c, tcn * 512:(tcn + 1) * 512], hynw[:, c:c + 1], rstdb[:, tcn * 512:(tcn + 1) * 512],
                    ALU.mult, ALU.mult, zres + ["hynw", ("rstdb", tcn)], [("yT", "h", c, tcn)])
        DUMP("yTh", yT[:, 0:4, :], [128, 4, L], [("yT", "h", c, t) for c in range(4) for t in range(4)])
        S.barrier()
        A.reset(PERS)
        if stop_after <= 6:
            S.emit(st)
            return nc, dbg_specs, S

        xaccF = A.alloc(16 * 1056).rearrange("p (i d) -> p i d", i=16)
        xacc = xaccF[:, :, 0:1024]
        ext_s = xaccF[:, :, 1024:1056]
        cnt_i = A.alloc(64).bitcast(mybir.dt.int32)
        tok_i = A.alloc(16).bitcast(mybir.dt.int32)
        P4 = A.mark()
        ext_n = ext_s
        wo_st = A.alloc(4 * 1024).rearrange("p (k n) -> p k n", k=4)
        wo = A.alloc(8 * 1024, BF16).rearrange("p (k n) -> p k n", k=8)
        rw = A.alloc(8 * 20).rearrange("p (k n) -> p k n", k=8)
        rw_bf = A.alloc(8 * 20, BF16).rearrange("p (k n) -> p k n", k=8)
        rbb = A.alloc(20)
        junk = A.alloc(1024)
        xsb = [A.alloc(1024, BF16) for _ in range(2)]
        ss = A.alloc(16); rstd = A.alloc(16); rtmp = A.alloc(16)
        rsm = A.alloc(256)
        wo_v = T["w_out"].rearrange("(k p) n -> p k n", p=128)
        for k in range(8):
            S.dma(wo_st[:, k % 4, :], wo_v[:, k, :], writes=[("wo_st", k % 4)])
            TT(wo[:, k, :], wo_st[:, k % 4, :], g1b, ALU.mult, [("wo_st", k % 4)], [("wo", k)], eng="pool")
        for i in range(16):
            S.dma(xacc[:, i, :], x_v[:, i, :], writes=[("xacc", i)])
        S.dma(rw, T["rw"].rearrange("(k p) n -> p k n", p=128), writes=["rw"])
        S.dma(rbb, bass.AP(T["rb"].tensor, 0, [[0, 128], [1, 20]]), writes=["rbb"])
        CP(rw_bf, rw, ["rw"], ["rw_bf"])
        for i in range(16):
            for dh in range(2):
                bank = (i * 2 + dh) % 4
                lst = [(PS[bank][:, :], yT[:, k, i * 128:(i + 1) * 128], wo[:, k, dh * 512:(dh + 1) * 512], k == 0, k == 7) for k in range(8)]
                MMG(lst, [("wo", k) for k in range(8)], [psr(bank)])
                TT(xacc[:, i, dh * 512:(dh + 1) * 512], PS[bank][:, :], xacc[:, i, dh * 512:(dh + 1) * 512], ALU.add, [psr(bank), ("xacc", i)], [("xacc", i)])
        DUMP("x1", xacc, [128, 16, 1024], [("xacc", i) for i in range(16)])

        def x1_tile(i):
            return xacc[:, i, :], [("xacc", i)]
        norm_to_T(x1_tile, 16, s2, b2, hT, "h2", ["s2", "b2"], junk, xsb, ss, rstd, rtmp)
        DUMP("h2T", hT, [128, 8, L], [("h2T", k, g) for k in range(8) for g in range(4)])
        ra = lambda n_: A.alloc(n_)
        lgA = ra(320).rearrange("p (i c) -> p i c", c=20)
        le_c = ra(256); mxA = ra(16); ohA = ra(64); d4 = ra(64); smA = ra(16); pselA = ra(16)
        tmA = ra(256); le4 = ra(64); m1A = ra(16); mskA = ra(64); le4b = ra(64); m2A = ra(16); selA = ra(64)
        e4A = ra(64); s4A = ra(16); rpA = ra(16); pwA = ra(64)
        v3 = lambda ap: ap.rearrange("p (i c) -> p i c", i=16)
        bc4 = lambda ap: bview(ap, 0, [[1, 16], [0, 4]])
        for i in range(16):
            lst = [(PS[4][:, i * 20:(i + 1) * 20], hT[:, k, i * 128:(i + 1) * 128], rw_bf[:, k, :], k == 0, k == 7) for k in range(8)]
            MMG(lst, [("h2T", k, i // 4) for k in range(8)] + ["rw_bf"], [psr(4)])
        R = ["rt"]
        TT(lgA, PS[4][:, 0:320].rearrange("p (i c) -> p i c", c=20), bview(rbb, 0, [[0, 16], [1, 20]]), ALU.add, [psr(4), "rbb"], R)
        S.dve(lambda e: e.tensor_reduce(out=mxA, in_=lgA[:, :, 0:4], axis=AX.X, op=ALU.max), R, R)
        TT(v3(ohA), lgA[:, :, 0:4], bc4(mxA), ALU.is_ge, R, R)
        TT(v3(d4), lgA[:, :, 0:4], bc4(mxA), ALU.subtract, R, R)
        ACT(d4, d4, AF.Exp, R, R)
        S.dve(lambda e: e.tensor_reduce(out=smA, in_=v3(d4), axis=AX.X, op=ALU.add), R, R)
        S.dve(lambda e: e.reciprocal(out=pselA, in_=smA), R, R)
        CP(v3(le_c), lgA[:, :, 4:20], R, R)
        TT(tmA.rearrange("p (q e) -> p q e", e=4), le_c.rearrange("p (q e) -> p q e", e=4), bview(ohA, 0, [[1, 64], [0, 4]]), ALU.mult, R, R)
        tm3 = v3(tmA)
        TT(v3(le4), tm3[:, :, 0:4], tm3[:, :, 4:8], ALU.add, R, R)
        TT(v3(le4), v3(le4), tm3[:, :, 8:12], ALU.add, R, R)
        TT(v3(le4), v3(le4), tm3[:, :, 12:16], ALU.add, R, R)
        S.dve(lambda e: e.tensor_reduce(out=m1A, in_=v3(le4), axis=AX.X, op=ALU.max), R, R)
        TT(v3(mskA), v3(le4), bc4(m1A), ALU.is_ge, R, R)
        STT(le4b, mskA, -1e30, le4, ALU.mult, ALU.add, R, R)
        S.dve(lambda e: e.tensor_reduce(out=m2A, in_=v3(le4b), axis=AX.X, op=ALU.max), R, R)
        TT(v3(selA), v3(le4), bc4(m2A), ALU.is_ge, R, R)
        TT(v3(e4A), v3(le4), bc4(m1A), ALU.subtract, R, R)
        ACT(e4A, e4A, AF.Exp, R, R)
        TT(e4A, e4A, selA, ALU.mult, R, R)
        S.dve(lambda e: e.tensor_reduce(out=s4A, in_=v3(e4A), axis=AX.X, op=ALU.add), R, R)
        S.dve(lambda e: e.reciprocal(out=rpA, in_=s4A), R, R)
        TT(rpA, rpA, pselA, ALU.mult, R, R)
        TT(v3(pwA), v3(e4A), bc4(rpA), ALU.mult, R, R)
        for g in range(4):
            TT(ext_n[:, :, g * 4:(g + 1) * 4], v3(pwA), bview(ohA, g, [[4, 16], [0, 4]]), ALU.mult, R, R + [("ext", g)])
        CP(ext_n[:, :, 16:20], v3(ohA), R, R + [("ext", 4)])
        EXT = [("ext", i) for i in range(16)]
        tokid_t = A.alloc(16)
        S.dma(tokid_t, T["tokid"], writes=["tokid_t"])
        CP(ext_n[:, :, 20], tokid_t, ["tokid_t"], ["ext_tok"])
        S.dve(lambda e: e.memset(ext_n[:, :, 21:32], 0.0), writes=["ext_pad"])
        EXT = EXT + ["ext_tok", "ext_pad"]
        DUMP("comb", ext_n[:, :, 0:16], [128, 16, 16], EXT)
        tri_f = A.alloc(128); tri_b = A.alloc(128, BF16)
        oh_bf = A.alloc(64, BF16).rearrange("p (i g) -> p i g", g=4)
        cnt_sb = A.alloc(64).rearrange("p (i g) -> p i g", g=4)
        tp = A.alloc(64).rearrange("p (i g) -> p i g", g=4)
        off = A.alloc(64).rearrange("p (i g) -> p i g", g=4)
        tot = A.alloc(4); gbase = A.alloc(4); gend = A.alloc(4)
        posf = A.alloc(16)
        pos_i = A.alloc(16).bitcast(mybir.dt.int32)
        i128 = A.alloc(64).rearrange("p (i g) -> p i g", g=4)
        lo_t = A.alloc(64).rearrange("p (i g) -> p i g", g=4)
        hi_t = A.alloc(64).rearrange("p (i g) -> p i g", g=4)
        S.dma(i128, T["i128"].rearrange("p (i g) -> p i g", g=4), writes=["i128"])
        S.pool(lambda e: e.affine_select(out=tri_f, in_=ones_f, pattern=[[1, 128]], compare_op=ALU.is_ge,
                                         fill=0.0, base=-1, channel_multiplier=-1), writes=["tri_f"])
        CP(tri_b, tri_f, ["tri_f"], ["tri_b"])
        CP(oh_bf, ext_n[:, :, 16:20], EXT, ["oh_bf"])
        MMG([(PS[6][:, i * 4:(i + 1) * 4], tri_b, oh_bf[:, i, :], True, True) for i in range(16)], ["tri_b", "oh_bf"], [psr(6)])
        MMG([(PS[7][:, i * 4:(i + 1) * 4], ones_bf, oh_bf[:, i, :], True, True) for i in range(16)], ["oh_bf"], [psr(7)])
        CP(cnt_sb, PS[7][:, 0:64].rearrange("p (i g) -> p i g", g=4), [psr(7)], ["cnt_sb"])
        S.dve(lambda e: e.memset(tp[:, 0, :], 0.0), writes=["tp"])
        for i in range(1, 16):
            TT(tp[:, i, :], tp[:, i - 1, :], cnt_sb[:, i - 1, :], ALU.add, ["tp", "cnt_sb"], ["tp"])
        TT(tot, tp[:, 15, :], cnt_sb[:, 15, :], ALU.add, ["tp", "cnt_sb"], ["tot"])
        S.dve(lambda e: e.memset(gbase[:, 0:1], 0.0), writes=["gbase"])
        for g in range(1, 4):
            TT(gbase[:, g:g + 1], gbase[:, g - 1:g], tot[:, g - 1:g], ALU.add, ["gbase", "tot"], ["gbase"])
        TT(gend, gbase, tot, ALU.add, ["gbase", "tot"], ["gend"])
        TT(off, PS[6][:, 0:64].rearrange("p (i g) -> p i g", g=4), tp, ALU.add, [psr(6), "tp"], ["off"])
        TT(off, off, bview(gbase, 0, [[0, 16], [1, 4]]), ALU.add, ["off", "gbase"], ["off"])
        TT(off, off, ext_n[:, :, 16:20], ALU.mult, ["off"] + EXT, ["off"])
        S.dve(lambda e: e.tensor_reduce(out=posf, in_=off, axis=AX.X, op=ALU.add), ["off"], ["posf"])
        CP(pos_i, posf, ["posf"], ["pos_i"])
        TT(lo_t, i128, bview(gbase, 0, [[0, 16], [1, 4]]), ALU.max, ["i128", "gbase"], ["lo_t"])
        TS(hi_t, i128, 128.0, None, ALU.add, None, ["i128"], ["hi_t"])
        TT(hi_t, hi_t, bview(gend, 0, [[0, 16], [1, 4]]), ALU.min, ["hi_t", "gend"], ["hi_t"])
        TT(hi_t, hi_t, lo_t, ALU.subtract, ["hi_t", "lo_t"], ["hi_t"])
        TS(hi_t, hi_t, 0.0, None, ALU.max, None, ["hi_t"], ["hi_t"])
        CP(cnt_i, hi_t.rearrange("p i g -> p (i g)"), ["hi_t"], ["cnt_i"])
        DUMP("posf", posf, [128, 16], ["posf"])
        scr_x = nc.dram_tensor("scr_x", [L, 1056], F32, kind="Internal").ap()
        for i in range(16):
            S.add("pool", lambda e, i=i: e.indirect_dma_start(out=scr_x[:, :], out_offset=bass.IndirectOffsetOnAxis(ap=pos_i[:, i:i + 1], axis=0),
                                                              in_=xaccF[:, i, :], in_offset=None),
                  [("xacc", i), "pos_i"] + EXT, [("scr_x", i)], dma=True)
        SCR = [("scr_x", i) for i in range(16)]
        sx_v = scr_x.rearrange("(i p) d -> p i d", p=128)
        for i in range(16):
            S.dma(xaccF[:, i, :], sx_v[:, i, :], reads=SCR, writes=[("xacc", i), ("ext_s", i)] + (EXT if i == 15 else []))
        CP(tok_i, ext_s[:, :, 20], [("ext_s", i) for i in range(16)], ["tok_i"])
        DUMP("x1s", xacc, [128, 16, 1024], [("xacc", i) for i in range(16)])
        DUMP("ext_s", ext_s, [128, 16, 32], [("ext_s", i) for i in range(16)])
        norm_to_T(x1_tile, 16, s2, b2, hT, "h2", ["s2", "b2"], junk, xsb, ss, rstd, rtmp)
        S.barrier()
        A.reset(P4)
        if stop_after <= 7:
            S.emit(st)
            return nc, dbg_specs, S

        w1b = [A.alloc(8 * 512, BF16).rearrange("p (k n) -> p k n", k=8) for _ in range(2)]
        w3b = [A.alloc(8 * 512, BF16).rearrange("p (k n) -> p k n", k=8) for _ in range(2)]
        w2st = A.alloc(4 * 1024).rearrange("p (k n) -> p k n", k=4)
        sa_t = [A.alloc(512) for _ in range(2)]
        hid_t = [A.alloc(512, BF16) for _ in range(2)]
        hidT_t = [A.alloc(512, BF16).rearrange("p (k t) -> p k t", k=4) for _ in range(2)]
        yflat = yT.rearrange("p k t -> p (k t)")
        w2b = [yflat[:, i * 4096:(i + 1) * 4096].rearrange("p (k n) -> p k n", k=4) for i in range(2)]
        w1_v = T["exp_w1"].rearrange("e (k p) n -> e p k n", p=128)
        w3_v = T["exp_w3"].rearrange("e (k p) n -> e p k n", p=128)
        w2_v = T["exp_w2"].rearrange("e (k p) n -> e p k n", p=128)

        def load_expert(e):
            s = e % 2
            for hk in range(2):
                S.dma(w1b[s][:, hk * 4:(hk + 1) * 4, :], w1_v[e][:, hk * 4:(hk + 1) * 4, :], writes=[("w1", s, hk)], eng="pool")
            for hk in range(2):
                S.dma(w3b[s][:, hk * 4:(hk + 1) * 4, :], w3_v[e][:, hk * 4:(hk + 1) * 4, :], writes=[("w3", s, hk)], eng="pool")
            S.dma(w2st, w2_v[e], writes=["w2st"])
            for k in range(4):
                TT(w2b[s][:, k, :], w2st[:, k, :], g2b, ALU.mult, ["w2st"], [("w2", s)], eng="pool")

        blocks = [(e, i) for e in range(16) for i in range(16)]

        def stage1(b):
            e, i = blocks[b]
            s, g, par = e % 2, e // 4, b % 2
            pa, pu = par, 2 + par
            if i == 2 and e + 1 < 16:
                load_expert(e + 1)
            if i == 0 and e % 4 == 0:
                for en in S.cond_engs:
                    S.regload(en, "g%d" % (g % 2), bview(cnt_i[0:1, :], g, [[4, 16]]), 16)
            S.cond_begin(("g%d" % (g % 2), i))
            lst = [(PS[pa][:, :], hT[:, k, i * 128:(i + 1) * 128], w1b[s][:, k, :], k == 0, k == 7) for k in range(8)]
            MMG(lst, [("w1", s, 0), ("w1", s, 1)], [psr(pa)])
            lst = [(PS[pu][:, :], hT[:, k, i * 128:(i + 1) * 128], w3b[s][:, k, :], k == 0, k == 7) for k in range(8)]
            MMG(lst, [("w3", s, 0), ("w3", s, 1)], [psr(pu)])
            ACT(sa_t[par], PS[pa][:, :], AF.Silu, [psr(pa)], [("sa", par)])
            STT(hid_t[par], PS[pu][:, :], ext_s[:, i, e:e + 1], sa_t[par], ALU.mult, ALU.mult, [psr(pu), ("sa", par)], [("hid", par)])
            S.cond_end()

        def stage2(b):
            e, i = blocks[b]
            par = b % 2
            S.cond_begin(("g%d" % ((e // 4) % 2), i))
            lst = [(PSB[4][:, par * 512 + hc * 128:par * 512 + (hc + 1) * 128], hid_t[par][:, hc * 128:(hc + 1) * 128], ident) for hc in range(4)]
            TRG(lst, [("hid", par)], [psr(4)])
            ACT(hidT_t[par], PSB[4][:, par * 512:(par + 1) * 512].rearrange("p (k t) -> p k t", k=4), AF.Copy, [psr(4)], [("hidT", par)])
            S.cond_end()

        def stage3(b):
            e, i = blocks[b]
            s, g, par = e % 2, e // 4, b % 2
            py0, py1 = 5, 6
            S.cond_begin(("g%d" % (g % 2), i))
            for dh, py in ((0, py0), (1, py1)):
                lst = [(PS[py][:, :], hidT_t[par][:, k, :], w2b[s][:, k, dh * 512:(dh + 1) * 512], k == 0, k == 3) for k in range(4)]
                MMG(lst, [("hidT", par), ("w2", s)], [psr(py)])
            for dh, py in ((0, py0), (1, py1)):
                STT(xacc[:, i, dh * 512:(dh + 1) * 512], PS[py][:, :], ext_s[:, i, 16 + g:17 + g], xacc[:, i, dh * 512:(dh + 1) * 512],
                    ALU.mult, ALU.add, [psr(py), ("xacc", i)], [("xacc", i)])
            S.cond_end()

        dummy = A.alloc(1200)
        dcnt = [0]

        def dcol():
            dcnt[0] += 1
            return dummy[0:1, dcnt[0] - 1:dcnt[0]]
        S.else_fn = {"pe": lambda eo: eo.matmul(PS[7][0:1, 0:1], lhsT=ident[0:1, 0:1], rhs=ident[0:1, 0:1], start=True, stop=True),
                     "dve": lambda eo: eo.memset(dcol(), 0.0),
                     "act": lambda eo: eo.activation(out=dcol(), in_=ones_f[0:1, 0:1], func=AF.Copy)}
        S.cond_engs = ("pe", "dve")
        load_expert(0)
        NB = len(blocks)
        for step in range(NB + 2):
            if step < NB:
                stage1(step)
            if 0 <= step - 1 < NB:
                stage2(step - 1)
            if 0 <= step - 2 < NB:
                stage3(step - 2)
        S.cond_engs = ("pe",)
        DUMP("x2", xacc, [128, 16, 1024], [("xacc", i) for i in range(16)])

        ssf = A.alloc(16); rstdf = A.alloc(16); rtmpf = A.alloc(16)
        junkf = sa_t[0]
        for i in range(16):
            for hh in range(2):
                ACT(junkf, xacc[:, i, hh * 512:(hh + 1) * 512], AF.Square, [("xacc", i)], [("sa", 0), ("ssf", i, hh)], accum_out=rtmpf[:, i:i + 1] if hh else ssf[:, i:i + 1])
            TT(ssf[:, i:i + 1], ssf[:, i:i + 1], rtmpf[:, i:i + 1], ALU.add, [("ssf", i, 0), ("ssf", i, 1)], [("ssf", i)])
            TS(ssf[:, i:i + 1], ssf[:, i:i + 1], 1.0 / D, EPS, ALU.mult, ALU.add, [("ssf", i)], [("ssf", i)])
            ACT(ssf[:, i:i + 1], ssf[:, i:i + 1], AF.Sqrt, [("ssf", i)], [("ssf", i)])
            S.dve(lambda e, i=i: e.reciprocal(out=rstdf[:, i:i + 1], in_=ssf[:, i:i + 1]), [("ssf", i)], [("rstdf", i)])
            STT(xacc[:, i, :], xacc[:, i, :], rstdf[:, i:i + 1], fnwb, ALU.mult, ALU.mult, [("xacc", i), ("rstdf", i), "fnwb"], [("xacc", i)])
            S.add("pool", lambda e, i=i: e.indirect_dma_start(out=out_d[:, :], out_offset=bass.IndirectOffsetOnAxis(ap=tok_i[:, i:i + 1], axis=0),
                                                              in_=xacc[:, i, :], in_offset=None),
                  [("xacc", i)], [("out", i)], dma=True)
        S.emit(st)
    return nc, dbg_specs, S


def make_in_maps(inp):
    f = lambda a: np.ascontiguousarray(np.asarray(a, dtype=np.float32))
    C = host_constants()
    fm = lambda v, nch: np.ascontiguousarray(f(v).reshape(nch, 128).T)
    shared = {
        "ada_w": f(inp["ada_w"][0]), "ada_b": f(inp["ada_b"][0]), "adab_fm": fm(inp["ada_b"][0], 48),
        "nw1": fm(inp["norm1_w"][0], 8), "nw2": fm(inp["norm2_w"][0], 8), "fnw": f(inp["final_norm_w"]),
        "w_in": f(inp["w_in"][0]),
        "convw": np.ascontiguousarray(f(inp["hy_conv_w"][0]).reshape(3, 12, 128).transpose(2, 1, 0)),
        "convb": fm(inp["hy_conv_b"][0], 12),
        "f_w1": f(inp["hy_f_w1"][0]), "f_w2": f(inp["hy_f_w2"][0]), "f_w3": f(inp["hy_f_w3"][0]), "f_wout": f(inp["hy_f_wout"][0]),
        "f_b": np.ascontiguousarray(np.stack([f(inp["hy_f_b1"][0]), f(inp["hy_f_b2"][0]), f(inp["hy_f_b3"][0])], 1)),
        "f_freq": f(inp["hy_f_freq"][0]).reshape(64, 1),
        "hy_skip": f(inp["hy_skip"][0]).reshape(1, 512), "hynw": fm(inp["hy_out_norm"][0], 4), "gnw": f(inp["ret_gn_w"][0]),
        "w_out": f(inp["w_out"][0]),
        "rw": np.ascontiguousarray(np.concatenate([f(inp["router_g_w"][0]), f(inp["router_e_w"][0])], 1)),
        "rb": np.ascontiguousarray(np.concatenate([f(inp["router_g_b"][0]), f(inp["router_e_b"][0])], 0)),
        "exp_w1": f(inp["exp_w1"][0]).reshape(16, D, 512), "exp_w3": f(inp["exp_w3"][0]).reshape(16, D, 512),
        "exp_w2": f(inp["exp_w2"][0]).reshape(16, 512, D),
    }
    for name, shape, dt in CONST_SPECS:
        shared["k_" + name] = C[name]
    x = f(inp["x"]); ctx = f(inp["ctx"]); c = f(inp["c"]); c_ctx = f(inp["c_ctx"])
    maps = []
    for b in range(8):
        m = dict(shared)
        m["x"] = x[b]
        m["ctx"] = ctx[b]
        m["cc"] = np.ascontiguousarray(np.stack([c[b].reshape(8, 128).T, c_ctx.reshape(8, 128).T], 2))
        maps.append(m)
    return maps


_NC = None


def kernel(**inputs):
    global _NC
    if _NC is None:
        _NC = build()[0]
    maps = make_in_maps(inputs)
    res = run_bass_kernel_spmd(_NC, maps, core_ids=list(range(8)))
    return np.stack([np.asarray(r["out"], dtype=np.float32) for r in res.results], 0)
```
